# Optimizing a Trainium2 kernel written in Bass

```python
import jax, jax.numpy as jnp
from jax import lax
import numpy as np

D_MODEL = 1024
BATCH = 8
SEQ = 4096
DEPTH = 2

GRID_W = 64
NA_HEADS = 4
NA_HEAD_DIM = 64
NA_WIN_ROWS = 8
NA_WIN_COLS = 16
MLA_HEADS = 8
MLA_Q_RANK = 384
MLA_KV_RANK = 256
MLA_NOPE_DIM = 64
MLA_ROPE_DIM = 32
MLA_V_DIM = 64
MLA_Q_BLOCK = 128
SWA_HEADS = 4
SWA_KV_HEADS = 2
SWA_HEAD_DIM = 64
SWA_WINDOW = 128
SWA_BLOCK = 128
N_EXPERTS = 16
EXPERT_FF = 512
EC_CAPACITY_FACTOR = 2
PLE_DIM = 256

ROPE_THETA = 10000.0
NORM_EPS = 1e-6
NEG_INF = -1e30

NA_WIDTH = NA_HEADS * NA_HEAD_DIM
MLA_WIDTH = MLA_HEADS * MLA_V_DIM
SWA_WIDTH = SWA_HEADS * SWA_HEAD_DIM
SWA_KV_WIDTH = SWA_KV_HEADS * SWA_HEAD_DIM
MIX_WIDTH = NA_WIDTH + MLA_WIDTH + SWA_WIDTH
IN_SIZES = (NA_WIDTH, NA_WIDTH, NA_WIDTH, MLA_Q_RANK, MLA_KV_RANK, MLA_ROPE_DIM,
            SWA_WIDTH, SWA_KV_WIDTH, SWA_KV_WIDTH)
IN_WIDTH = sum(IN_SIZES)

kernel_name = 'hybrid_na_mla_swa_ec_encoder'


def rms_norm(x, g):
    xf = x.astype(jnp.float32)
    y = xf * lax.rsqrt(jnp.mean(xf * xf, axis=-1, keepdims=True) + NORM_EPS)
    return (y * g.astype(jnp.float32)).astype(x.dtype)


def rope_tables(pos, dim):
    inv = 1.0 / (ROPE_THETA ** (jnp.arange(0, dim, 2, dtype=jnp.float32) / dim))
    ang = pos.astype(jnp.float32)[:, None] * inv[None, :]
    return jnp.cos(ang), jnp.sin(ang)


def apply_rope(x, cos, sin):
    d2 = x.shape[-1] // 2
    x1 = x[..., :d2].astype(jnp.float32)
    x2 = x[..., d2:].astype(jnp.float32)
    c = cos[None, :, None, :]
    s = sin[None, :, None, :]
    return jnp.concatenate([x1 * c - x2 * s, x2 * c + x1 * s], axis=-1).astype(x.dtype)


def neighbourhood_attention(q, k, v, rpb):
    B, S, H, hd = q.shape
    rows = S // GRID_W
    wr = min(NA_WIN_ROWS, rows)
    wc = NA_WIN_COLS
    r = jnp.arange(rows)
    c = jnp.arange(GRID_W)
    r0 = jnp.clip(r - wr // 2, 0, rows - wr)
    c0 = jnp.clip(c - wc // 2, 0, GRID_W - wc)
    key_rows = r0[:, None] + jnp.arange(wr)[None, :]
    qg = q.reshape(B, rows, GRID_W, H, hd)
    kg = k.reshape(B, rows, GRID_W, H, hd)[:, key_rows]
    vg = v.reshape(B, rows, GRID_W, H, hd)[:, key_rows]
    s = jnp.einsum('brchd,briwhd->bhrciw', qg, kg,
                   preferred_element_type=jnp.float32) * (hd ** -0.5)
    kc = jnp.arange(GRID_W)
    col_ok = (kc[None, :] >= c0[:, None]) & (kc[None, :] < c0[:, None] + wc)
    dr = key_rows - r[:, None]
    dc = jnp.clip(kc[None, :] - c[:, None], -(wc - 1), wc - 1)
    bias = rpb[:, dr[:, None, :, None] + NA_WIN_ROWS - 1,
               dc[None, :, None, :] + NA_WIN_COLS - 1]
    s = s + bias.astype(jnp.float32)[None]
    s = jnp.where(col_ok[:, None, :], s, NEG_INF)
    p = jax.nn.softmax(s.reshape(B, H, rows, GRID_W, wr * GRID_W), axis=-1).reshape(s.shape)
    o = jnp.einsum('bhrciw,briwhd->brchd', p.astype(v.dtype), vg)
    return o.reshape(B, S, H * hd)


def latent_attention(c_q, c_kv, k_rope, q_norm, w_q_up, kv_norm, w_kv_up, cos, sin):
    B, S, _ = c_q.shape
    H = MLA_HEADS
    q = (rms_norm(c_q, q_norm) @ w_q_up).reshape(B, S, H, MLA_NOPE_DIM + MLA_ROPE_DIM)
    q_nope = q[..., :MLA_NOPE_DIM]
    q_rope = apply_rope(q[..., MLA_NOPE_DIM:], cos, sin)
    kv = (rms_norm(c_kv, kv_norm) @ w_kv_up).reshape(B, S, H, MLA_NOPE_DIM + MLA_V_DIM)
    k_nope = kv[..., :MLA_NOPE_DIM]
    v = kv[..., MLA_NOPE_DIM:]
    k_r = apply_rope(k_rope[:, :, None, :], cos, sin)[:, :, 0]
    scale = (MLA_NOPE_DIM + MLA_ROPE_DIM) ** -0.5
    nb = S // MLA_Q_BLOCK

    def to_blocks(t):
        return jnp.moveaxis(t.reshape(B, nb, MLA_Q_BLOCK, *t.shape[2:]), 1, 0)

    def block_attn(args):
        qn, qr = args
        s = (jnp.einsum('bqhd,bkhd->bhqk', qn, k_nope, preferred_element_type=jnp.float32)
             + jnp.einsum('bqhr,bkr->bhqk', qr, k_r, preferred_element_type=jnp.float32)) * scale
        p = jax.nn.softmax(s, axis=-1)
        return jnp.einsum('bhqk,bkhd->bqhd', p.astype(v.dtype), v)

    o = lax.map(block_attn, (to_blocks(q_nope), to_blocks(q_rope)))
    return jnp.moveaxis(o, 0, 1).reshape(B, S, H * MLA_V_DIM)


def window_gqa_sink(q, k, v, sink, cos, sin):
    B, S, H, hd = q.shape
    KVH = k.shape[2]
    G = H // KVH
    L = SWA_BLOCK
    nb = S // L
    q = apply_rope(q, cos, sin)
    k = apply_rope(k, cos, sin)
    pad = ((0, 0), (L, L), (0, 0), (0, 0))
    kp = jnp.pad(k, pad).reshape(B, nb + 2, L, KVH, hd)
    vp = jnp.pad(v, pad).reshape(B, nb + 2, L, KVH, hd)
    kb = jnp.concatenate([kp[:, :-2], kp[:, 1:-1], kp[:, 2:]], axis=2)
    vb = jnp.concatenate([vp[:, :-2], vp[:, 1:-1], vp[:, 2:]], axis=2)
    qb = q.reshape(B, nb, L, KVH, G, hd)
    s = jnp.einsum('bnqkgd,bnjkd->bkgnqj', qb, kb,
                   preferred_element_type=jnp.float32) * (hd ** -0.5)
    qi = jnp.arange(L)[:, None]
    kj = jnp.arange(3 * L)[None, :]
    rel = kj - L - qi
    kpos = (jnp.arange(nb)[:, None, None] - 1) * L + kj[None]
    valid = (jnp.abs(rel)[None] <= SWA_WINDOW) & (kpos >= 0) & (kpos < S)
    s = jnp.where(valid, s, NEG_INF)
    sink_l = sink.astype(jnp.float32).reshape(KVH, G)[None, :, :, None, None, None]
    m = jnp.maximum(jnp.max(s, axis=-1, keepdims=True), sink_l)
    e = jnp.exp(s - m)
    p = e / (jnp.sum(e, axis=-1, keepdims=True) + jnp.exp(sink_l - m))
    o = jnp.einsum('bkgnqj,bnjkd->bnqkgd', p.astype(v.dtype), vb)
    return o.reshape(B, S, H * hd)


def expert_choice_ffn(h, w_router, w_gate, w_up, w_down):
    B, S, D = h.shape
    cap = EC_CAPACITY_FACTOR * S // N_EXPERTS
    logits = jnp.einsum('bsd,de->bse', h, w_router, preferred_element_type=jnp.float32)
    aff = jax.nn.softmax(logits, axis=-1)
    g, idx = lax.top_k(jnp.swapaxes(aff, 1, 2), cap)
    xs = jax.vmap(lambda hb, ib: hb[ib])(h, idx)
    a = jnp.einsum('becd,edf->becf', xs, w_gate)
    u = jnp.einsum('becd,edf->becf', xs, w_up)
    y = jnp.einsum('becf,efd->becd', jax.nn.silu(a) * u, w_down)
    y = y * g[..., None].astype(y.dtype)
    return jax.vmap(lambda ib, yb: jnp.zeros((S, D), yb.dtype)
                    .at[ib.reshape(-1)].add(yb.reshape(-1, D)))(idx, y)


def setup_inputs(seed: int = 0) -> dict:
    key = jax.random.key(seed)
    ks = jax.random.split(key, 24)
    f32 = jnp.float32

    def nrm(k, shape, scale):
        return jax.random.normal(k, shape, f32) * scale

    def gain(k, shape):
        return 1.0 + 0.05 * jax.random.normal(k, shape, f32)

    return {
        'x': nrm(ks[0], (BATCH, SEQ, D_MODEL), 1.0),
        'p': nrm(ks[1], (DEPTH, BATCH, SEQ, PLE_DIM), 1.0),
        'attn_norm': gain(ks[2], (DEPTH, D_MODEL)),
        'w_in': nrm(ks[3], (DEPTH, D_MODEL, IN_WIDTH), D_MODEL ** -0.5),
        'na_rpb': nrm(ks[4], (DEPTH, NA_HEADS, 2 * NA_WIN_ROWS - 1, 2 * NA_WIN_COLS - 1), 0.1),
        'mla_q_norm': gain(ks[5], (DEPTH, MLA_Q_RANK)),
        'mla_w_q_up': nrm(ks[6], (DEPTH, MLA_Q_RANK, MLA_HEADS * (MLA_NOPE_DIM + MLA_ROPE_DIM)), MLA_Q_RANK ** -0.5),
        'mla_kv_norm': gain(ks[7], (DEPTH, MLA_KV_RANK)),
        'mla_w_kv_up': nrm(ks[8], (DEPTH, MLA_KV_RANK, MLA_HEADS * (MLA_NOPE_DIM + MLA_V_DIM)), MLA_KV_RANK ** -0.5),
        'swa_sink': nrm(ks[9], (DEPTH, SWA_HEADS), 1.0),
        'w_out': nrm(ks[10], (DEPTH, MIX_WIDTH, D_MODEL), MIX_WIDTH ** -0.5),
        'ffn_norm': gain(ks[11], (DEPTH, D_MODEL)),
        'w_router': nrm(ks[12], (DEPTH, D_MODEL, N_EXPERTS), D_MODEL ** -0.5),
        'w_gate_e': nrm(ks[13], (DEPTH, N_EXPERTS, D_MODEL, EXPERT_FF), D_MODEL ** -0.5),
        'w_up_e': nrm(ks[14], (DEPTH, N_EXPERTS, D_MODEL, EXPERT_FF), D_MODEL ** -0.5),
        'w_down_e': nrm(ks[15], (DEPTH, N_EXPERTS, EXPERT_FF, D_MODEL), EXPERT_FF ** -0.5),
        'ple_norm': gain(ks[16], (DEPTH, D_MODEL)),
        'w_ple_gate': nrm(ks[17], (DEPTH, D_MODEL, D_MODEL), D_MODEL ** -0.5),
        'w_ple_proj': nrm(ks[18], (DEPTH, PLE_DIM, D_MODEL), PLE_DIM ** -0.5),
        'final_norm': gain(ks[19], (D_MODEL,)),
    }


def reference(x, p, attn_norm, w_in, na_rpb, mla_q_norm, mla_w_q_up, mla_kv_norm, mla_w_kv_up,
              swa_sink, w_out, ffn_norm, w_router, w_gate_e, w_up_e, w_down_e,
              ple_norm, w_ple_gate, w_ple_proj, final_norm):
    B, S, D = x.shape
    pos = jnp.arange(S)
    cos_mla, sin_mla = rope_tables(pos, MLA_ROPE_DIM)
    cos_swa, sin_swa = rope_tables(pos, SWA_HEAD_DIM)
    offsets = [int(o) for o in np.cumsum(IN_SIZES)[:-1]]
    h = x
    for i in range(DEPTH):
        hn = rms_norm(h, attn_norm[i])
        proj = hn @ w_in[i]
        na_q, na_k, na_v, mla_cq, mla_ckv, mla_kr, swa_q, swa_k, swa_v = jnp.split(proj, offsets, axis=-1)
        o_na = neighbourhood_attention(
            na_q.reshape(B, S, NA_HEADS, NA_HEAD_DIM),
            na_k.reshape(B, S, NA_HEADS, NA_HEAD_DIM),
            na_v.reshape(B, S, NA_HEADS, NA_HEAD_DIM), na_rpb[i])
        o_mla = latent_attention(mla_cq, mla_ckv, mla_kr, mla_q_norm[i], mla_w_q_up[i],
                                 mla_kv_norm[i], mla_w_kv_up[i], cos_mla, sin_mla)
        o_swa = window_gqa_sink(
            swa_q.reshape(B, S, SWA_HEADS, SWA_HEAD_DIM),
            swa_k.reshape(B, S, SWA_KV_HEADS, SWA_HEAD_DIM),
            swa_v.reshape(B, S, SWA_KV_HEADS, SWA_HEAD_DIM), swa_sink[i], cos_swa, sin_swa)
        h = h + jnp.concatenate([o_na, o_mla, o_swa], axis=-1) @ w_out[i]
        h = h + expert_choice_ffn(rms_norm(h, ffn_norm[i]), w_router[i],
                                  w_gate_e[i], w_up_e[i], w_down_e[i])
        gate = jax.nn.sigmoid(rms_norm(h, ple_norm[i]) @ w_ple_gate[i])
        h = h + gate * (p[i] @ w_ple_proj[i])
    return rms_norm(h, final_norm)
```

```python
import numpy as np
import concourse.bass as bass
import concourse.mybir as mybir
from contextlib import ExitStack

F32 = mybir.dt.float32
BF16 = mybir.dt.bfloat16
I32 = mybir.dt.int32
ALU = mybir.AluOpType
AF = mybir.ActivationFunctionType
AX = mybir.AxisListType

ENGS = ("pe", "act", "dve", "pool", "sp")
N_DMA_SEMS = 32
SB_BASE = 16512
DBG_CUT = None
SB_END = 229312
PF1_BASE = SB_END - 32768
PF2_BASE = SB_END - 65536


class Buf:
    __slots__ = ("t", "w", "r", "name")

    def __init__(self, t, name=""):
        self.t = t
        self.w = None
        self.r = {}
        self.name = name

    def __getitem__(self, idx):
        return self.t[idx]


class PFView(Buf):
    __slots__ = ("base",)

    def __init__(self, base, ap, name=""):
        self.base = base
        self.t = ap
        self.name = name

    w = property(lambda s: s.base.w, lambda s, v: setattr(s.base, "w", v))
    r = property(lambda s: s.base.r, lambda s, v: setattr(s.base, "r", v))


class Prog:
    def __init__(self, nc, es):
        self.nc = nc
        self.es = es
        self.q = {e: [] for e in ENGS}
        self.cnt = {e: 0 for e in ENGS}
        self.sems = {}
        for e in ENGS:
            self.sems[e] = es.enter_context(nc.semaphore("s_" + e))
        self.dsem_cnt = []
        for i in range(N_DMA_SEMS):
            k = "d%d" % i
            self.sems[k] = es.enter_context(nc.semaphore("s_" + k))
            self.dsem_cnt.append(0)
        self.dnext = 0
        self.dnext2 = 0
        self.known = {e: {} for e in ENGS}
        self.sb_off = SB_BASE
        self.sb_hi = SB_BASE
        self.uid = 0
        self.n_inst = 0

    def sb(self, shape, dtype, name=None):
        self.uid += 1
        nm = "%s_%d" % (name or "sb", self.uid)
        esz = {F32: 4, BF16: 2, I32: 4, mybir.dt.int16: 2}[dtype]
        per = int(np.prod(shape[1:])) * esz
        off = (self.sb_off + 31) // 32 * 32
        t = self.nc.alloc_sbuf_tensor_at(nm, list(shape), dtype, offset=off)
        self.sb_off = off + per
        self.sb_hi = max(self.sb_hi, self.sb_off)
        assert self.sb_off <= PF1_BASE, ("SBUF overflow", self.sb_off)
        return Buf(t, nm)

    def mark(self):
        return self.sb_off

    def release(self, mark):
        self.sb_off = mark

    def _deps(self, eng, reads, writes):
        deps = {}

        def add(k, v):
            if deps.get(k, 0) < v:
                deps[k] = v
        for b in reads:
            if b.w is not None:
                add(*b.w)
        for b in writes:
            if b.w is not None:
                add(*b.w)
            for k, v in b.r.items():
                add(k, v)
        if eng == "pe":
            deps.pop("pe", None)
        out = []
        kn = self.known[eng]
        for k, v in deps.items():
            if kn.get(k, 0) < v:
                kn[k] = v
                out.append((k, v))
        return out

    def _emit_waits(self, eng, deps):
        for k, v in deps:
            s = self.sems[k]
            self.q[eng].append(lambda e, s=s, v=v: e.wait_ge(s, v))

    def op(self, eng, fn, reads=(), writes=()):
        if DBG_CUT is not None and self.n_inst >= DBG_CUT:
            return
        deps = self._deps(eng, reads, writes)
        self._emit_waits(eng, deps)
        self.cnt[eng] += 1
        val = self.cnt[eng]
        s = self.sems[eng]
        self.q[eng].append(lambda e, fn=fn, s=s: fn(e).then_inc(s, 1))
        for b in writes:
            b.w = (eng, val)
            b.r = {}
        for b in reads:
            if b.r.get(eng, 0) < val:
                b.r[eng] = val
        self.n_inst += 1

    def dma(self, eng, fn, reads=(), writes=()):
        if DBG_CUT is not None and self.n_inst >= DBG_CUT:
            return
        half = N_DMA_SEMS // 2
        if eng == "sp":
            i = self.dnext
            self.dnext = (self.dnext + 1) % half
        else:
            i = half + self.dnext2
            self.dnext2 = (self.dnext2 + 1) % half
        k = "d%d" % i
        deps = self._deps(eng, reads, writes)
        prev = self.dsem_cnt[i]
        if prev and self.known[eng].get(k, 0) < prev:
            self.known[eng][k] = prev
            deps.append((k, prev))
        self._emit_waits(eng, deps)
        self.dsem_cnt[i] += 16
        val = self.dsem_cnt[i]
        s = self.sems[k]
        self.q[eng].append(lambda e, fn=fn, s=s: fn(e).then_inc(s, 16))
        for b in writes:
            b.w = (k, val)
            b.r = {}
        for b in reads:
            if b.r.get(k, 0) < val:
                b.r[k] = val
        self.n_inst += 1

    def barrier(self):
        for e in ENGS:
            deps = []
            for e2 in ENGS:
                if e2 != e and self.cnt[e2] > self.known[e].get(e2, 0):
                    self.known[e][e2] = self.cnt[e2]
                    deps.append((e2, self.cnt[e2]))
            for i in range(N_DMA_SEMS):
                k = "d%d" % i
                v = self.dsem_cnt[i]
                if v > self.known[e].get(k, 0):
                    self.known[e][k] = v
                    deps.append((k, v))
            self._emit_waits(e, deps)

    def finish(self):
        nc = self.nc
        self.barrier()
        with nc.Block() as block:
            @block.tensor
            def _(e):
                for f in self.q["pe"]:
                    f(e)

            @block.scalar
            def _(e):
                for f in self.q["act"]:
                    f(e)

            @block.vector
            def _(e):
                for f in self.q["dve"]:
                    f(e)

            @block.gpsimd
            def _(e):
                for f in self.q["pool"]:
                    f(e)

            @block.sync
            def _(e):
                for f in self.q["sp"]:
                    f(e)


from concourse.bass_utils import run_bass_kernel_spmd

S = 4096
D = 1024
NT = 32
NCH = 8
EPS = 1e-6
NEG = -30000.0
DBG_NT = None
GR = [(0, 512), (512, 768), (768, 1152), (1152, 1440), (1440, 1952)]


class NS:
    pass


def _mm(P, out, lhsT, rhs, start, stop, reads, writes):
    P.op("pe", lambda e: e.matmul(out, lhsT=lhsT, rhs=rhs, start=start, stop=stop), reads, writes)


def _tr(P, out, in_, ident, reads, writes):
    P.op("pe", lambda e: e.transpose(out=out, in_=in_, identity=ident), reads, writes)


def _rstd(P, ss, rstd, n):
    P.op("act", lambda e: e.activation(out=rstd[:], in_=ss[:], func=AF.Ln, scale=1.0 / n, bias=EPS), [ss], [rstd])
    P.op("act", lambda e: e.activation(out=rstd[:], in_=rstd[:], func=AF.Exp, scale=-0.5), [rstd], [rstd])


def setup_regions(P, C):
    nc = P.nc
    t1 = nc.alloc_sbuf_tensor_at("pf1", [128, 16384], BF16, offset=PF1_BASE)
    t2 = nc.alloc_sbuf_tensor_at("pf2", [128, 16384], BF16, offset=PF2_BASE)
    C.pf1 = Buf(t1, "pf1")
    C.pf2 = Buf(t2, "pf2")
    C.v_w_in = PFView(C.pf2, t2[:, 0:15616].rearrange("p (c n) -> p c n", c=8), "v_w_in")
    C.v_w_out = PFView(C.pf1, t1[:, 0:8192].rearrange("p (c n) -> p c n", c=8), "v_w_out")
    C.v_w_r = PFView(C.pf1, t1[:, 8192:8320].rearrange("p (c n) -> p c n", c=8), "v_w_r")
    C.v_w_g = PFView(C.pf1, t1[:, 0:8192].rearrange("p (c n) -> p c n", c=8), "v_w_g")
    C.v_w_p = PFView(C.pf1, t1[:, 8192:10240].rearrange("p (c n) -> p c n", c=2), "v_w_p")
    C.v_wg0 = PFView(C.pf1, t1[:, 0:4096].rearrange("p (c n) -> p c n", c=8), "v_wg0")
    C.v_wu0 = PFView(C.pf1, t1[:, 4096:8192].rearrange("p (c n) -> p c n", c=8), "v_wu0")
    C.v_wd0 = PFView(C.pf1, t1[:, 8192:12288].rearrange("p (c n) -> p c n", c=4), "v_wd0")


def pre_w_in(P, C, l):
    v = C.v_w_in
    P.dma("pool", lambda e: e.dma_start(out=v[:], in_=C.w_in[l].rearrange("(c p) n -> p c n", p=128)), [], [v])


def pre_w_out(P, C, l):
    v, r = C.v_w_out, C.v_w_r
    P.dma("pool", lambda e: e.dma_start(out=v[:], in_=C.w_out[l].rearrange("(c p) n -> p c n", p=128)), [], [v])
    P.dma("pool", lambda e: e.dma_start(out=r[:], in_=C.w_router[l].rearrange("(c p) n -> p c n", p=128)), [], [r])


def pre_exp0(P, C, l):
    P.dma("pool", lambda e: e.dma_start(out=C.v_wg0[:], in_=C.w_gate_e[l, 0].rearrange("(c p) n -> p c n", p=128)), [], [C.v_wg0])
    P.dma("pool", lambda e: e.dma_start(out=C.v_wu0[:], in_=C.w_up_e[l, 0].rearrange("(c p) n -> p c n", p=128)), [], [C.v_wu0])
    P.dma("pool", lambda e: e.dma_start(out=C.v_wd0[:], in_=C.w_down_e[l, 0].rearrange("(c p) n -> p c n", p=128)), [], [C.v_wd0])


def pre_ple(P, C, l):
    P.dma("pool", lambda e: e.dma_start(out=C.v_w_g[:], in_=C.w_ple_gate[l].rearrange("(c p) n -> p c n", p=128)), [], [C.v_w_g])
    P.dma("pool", lambda e: e.dma_start(out=C.v_w_p[:], in_=C.w_ple_proj[l].rearrange("(c p) n -> p c n", p=128)), [], [C.v_w_p])


def setup_consts(P, C):
    K = NS()
    K.identf = P.sb([128, 128], F32, "identf")
    K.ident = P.sb([128, 128], BF16, "ident")
    K.ones = P.sb([128, 128], BF16, "ones")
    K.onesf = P.sb([128, 64], F32, "onesf")
    K.L = P.sb([128, 128], BF16, "Ltri")
    K.Lf = P.sb([128, 128], F32, "Lf")
    K.iota_i = P.sb([128, 512], I32, "iota_i")
    K.iota = P.sb([128, 512], F32, "iota")
    K.iota16 = P.sb([128, 512], mybir.dt.int16, "iota16")
    K.pcol_i = P.sb([128, 1], I32, "pcol_i")
    K.pcol = P.sb([128, 1], F32, "pcol")
    K.aff = P.sb([128, 32, 16], F32, "aff")
    K.idx = P.sb([128, 16, 4], I32, "idx")
    K.gsl = P.sb([128, 16, 4], F32, "gsl")
    P.op("pool", lambda e: e.memset(K.identf[:], 0.0), [], [K.identf])
    P.op("pool", lambda e: e.affine_select(out=K.identf[:], in_=K.identf[:], pattern=[[-1, 128]],
                                           compare_op=ALU.not_equal, fill=1.0, base=0, channel_multiplier=1),
         [K.identf], [K.identf])
    P.op("dve", lambda e: e.tensor_copy(out=K.ident[:], in_=K.identf[:]), [K.identf], [K.ident])
    P.op("dve", lambda e: e.memset(K.ones[:], 1.0), [], [K.ones])
    P.op("dve", lambda e: e.memset(K.onesf[:], 1.0), [], [K.onesf])
    P.op("pool", lambda e: e.memset(K.Lf[:], 0.0), [], [K.Lf])
    P.op("pool", lambda e: e.affine_select(out=K.Lf[:], in_=K.Lf[:], pattern=[[-1, 128]],
                                           compare_op=ALU.is_ge, fill=1.0, base=0, channel_multiplier=1),
         [K.Lf], [K.Lf])
    P.op("dve", lambda e: e.tensor_copy(out=K.L[:], in_=K.Lf[:]), [K.Lf], [K.L])
    P.op("pool", lambda e: e.iota(K.iota_i[:], pattern=[[1, 512]], base=0, channel_multiplier=0), [], [K.iota_i])
    P.op("dve", lambda e: e.tensor_copy(out=K.iota[:], in_=K.iota_i[:]), [K.iota_i], [K.iota])
    P.op("dve", lambda e: e.tensor_copy(out=K.iota16[:], in_=K.iota_i[:]), [K.iota_i], [K.iota16])
    P.op("pool", lambda e: e.iota(K.pcol_i[:], pattern=[[0, 1]], base=0, channel_multiplier=1), [], [K.pcol_i])
    P.op("dve", lambda e: e.tensor_copy(out=K.pcol[:], in_=K.pcol_i[:]), [K.pcol_i], [K.pcol])
    return K


def phase_A(P, C, K, l):
    mk = P.mark()
    ps, psb, bk = C.ps, C.psb, C.bk
    w_in = C.v_w_in
    g_at = P.sb([128, 1024], F32, "g_at")
    g_q = P.sb([128, 384], F32, "g_q")
    g_kv = P.sb([128, 256], F32, "g_kv")
    P.dma("sp", lambda e: e.dma_start(out=g_at[:], in_=C.attn_norm[l].partition_broadcast(128)), [], [g_at])
    P.dma("sp", lambda e: e.dma_start(out=g_q[:], in_=C.mla_q_norm[l].partition_broadcast(128)), [], [g_q])
    P.dma("sp", lambda e: e.dma_start(out=g_kv[:], in_=C.mla_kv_norm[l].partition_broadcast(128)), [], [g_kv])
    cos_sw = P.sb([128, 32, 32], F32, "cos_sw")
    sin_sw = P.sb([128, 32, 32], F32, "sin_sw")
    cos_ml = P.sb([128, 32, 16], F32, "cos_ml")
    sin_ml = P.sb([128, 32, 16], F32, "sin_ml")
    for t, src in ((cos_sw, C.cos_swa), (sin_sw, C.sin_swa), (cos_ml, C.cos_mla), (sin_ml, C.sin_mla)):
        P.dma("sp", lambda e, t=t, src=src: e.dma_start(out=t[:], in_=src.rearrange("(t p) d -> p t d", p=128)), [], [t])
    hts = [P.sb([128, 1024], F32, "ht") for _ in range(3)]
    junk = P.sb([128, 1024], BF16, "junk")
    xns = [P.sb([128, 1024], BF16, "xn") for _ in range(3)]
    xTs = [P.sb([128, 8, 128], BF16, "xT") for _ in range(2)]
    ss = [P.sb([128, 1], F32, "ss") for _ in range(3)]
    rs = [P.sb([128, 1], F32, "rstd") for _ in range(3)]
    ssq = P.sb([128, 1], F32, "ssq")
    rq = P.sb([128, 1], F32, "rq")
    sskv = P.sb([128, 1], F32, "sskv")
    rkv = P.sb([128, 1], F32, "rkv")
    naqk = P.sb([128, 512], BF16, "naqk")
    cqn = P.sb([128, 384], BF16, "cqn")
    ckvn = P.sb([128, 256], BF16, "ckvn")
    kr96 = P.sb([128, 96], BF16, "kr96")
    swqk = P.sb([128, 8, 64], BF16, "swqk")
    ra = P.sb([128, 384], F32, "ra")
    krt = P.sb([128, 2, 64], BF16, "krt")
    rb = P.sb([128, 384], F32, "rb")
    st = []
    for _ in range(2):
        s = NS()
        s.naqk = P.sb([128, 4, 512], BF16, "st_naqk")
        s.nav = P.sb([128, 4, 4, 65], BF16, "st_nav")
        s.cq = P.sb([128, 3, 512], BF16, "st_cq")
        s.ckv = P.sb([128, 2, 512], BF16, "st_ckv")
        s.kr = P.sb([128, 512], BF16, "st_kr")
        s.swqk = P.sb([128, 4, 512], BF16, "st_swqk")
        s.swv = P.sb([128, 4, 2, 65], BF16, "st_swv")
        P.op("pool", lambda e, s=s: e.memset(s.nav[:], 1.0), [], [s.nav])
        P.op("pool", lambda e, s=s: e.memset(s.swv[:], 1.0), [], [s.swv])
        st.append(s)
    P.op("pool", lambda e: e.memset(kr96[:], 0.0), [], [kr96])
    src = C.x if l == 0 else C.h_d
    srcB = C.B["x"] if l == 0 else C.B["h"]
    b5, b6, b7 = bk[5], bk[6], bk[7]

    def bf(bank, c0, c1):
        return psb[:, bank * 1024 + c0: bank * 1024 + c1]

    gss = [P.sb([128, 1056], F32, "gs") for _ in range(2)]
    naqks = [naqk, P.sb([128, 512], BF16, "naqk2")]
    NTT = DBG_NT or NT

    def fa(T):
        ht, xn = hts[T % 3], xns[T % 3]
        P.dma("sp", lambda e, ht=ht, T=T: e.dma_start(out=ht[:], in_=src[T * 128:(T + 1) * 128, :]), [srcB[T]], [ht])
        P.op("act", lambda e, ht=ht, T=T: e.activation(out=junk[:], in_=ht[:], func=AF.Square, accum_out=ss[T % 3][:]),
             [ht], [junk, ss[T % 3]])
        _rstd(P, ss[T % 3], rs[T % 3], 1024)
        P.op("dve", lambda e, ht=ht, xn=xn, T=T: e.scalar_tensor_tensor(out=xn[:], in0=ht[:], scalar=rs[T % 3][:], in1=g_at[:],
                                                                      op0=ALU.mult, op1=ALU.mult), [ht, rs[T % 3], g_at], [xn])

    def fb(T):
        xn, xT = xns[T % 3], xTs[T % 2]
        for c in range(8):
            _tr(P, bf(5, c * 128, (c + 1) * 128), xn[:, c * 128:(c + 1) * 128], K.ident[:], [xn, K.ident], [b5])
        P.op("act", lambda e, xT=xT: e.activation(out=xT[:].rearrange("p c t -> p (c t)"), in_=bf(5, 0, 1024), func=AF.Copy), [b5], [xT])

    def mm(T):
        xT = xTs[T % 2]
        for gi, (c0, c1) in enumerate(GR):
            for k in range(8):
                _mm(P, ps[:, gi * 512: gi * 512 + (c1 - c0)], xT[:, k, :], w_in[:, k, c0:c1], k == 0, k == 7, [xT, w_in], [bk[gi]])

    def gc(T):
        ch, tt = T // 4, T % 4
        s = st[ch % 2]
        gs = gss[T % 2]
        nq = naqks[T % 2]
        P.op("act", lambda e, nq=nq: e.activation(out=nq[:], in_=ps[:, 0:512], func=AF.Copy), [bk[0]], [nq])
        P.op("dve", lambda e, gs=gs: e.tensor_copy(out=gs[:, 0:384], in_=ps[:, 1024:1408]), [bk[2]], [gs])
        P.op("dve", lambda e, gs=gs: e.tensor_copy(out=gs[:, 672:1056], in_=ps[:, 2048:2432]), [bk[4]], [gs])
        P.op("act", lambda e, gs=gs: e.activation(out=gs[:, 384:672], in_=ps[:, 1536:1824], func=AF.Copy), [bk[3]], [gs])
        P.op("dve", lambda e, s=s, tt=tt: e.tensor_copy(out=s.nav[:, tt, :, 0:64], in_=ps[:, 512:768].rearrange("p (h d) -> p h d", h=4)), [bk[1]], [s.nav])
        P.op("act", lambda e, s=s, tt=tt: e.activation(out=s.swv[:, tt, :, 0:64], in_=ps[:, 2432:2560].rearrange("p (h d) -> p h d", h=2),
                                                       func=AF.Copy), [bk[4]], [s.swv])

    def post_a(T):
        gs = gss[T % 2]
        P.op("act", lambda e, gs=gs: e.activation(out=junk2[:, 0:384], in_=gs[:, 0:384], func=AF.Square, accum_out=ssq[:]), [gs], [junk2, ssq])
        _rstd(P, ssq, rq, 384)
        P.op("dve", lambda e, gs=gs: e.scalar_tensor_tensor(out=cqn[:], in0=gs[:, 0:384], scalar=rq[:], in1=g_q[:],
                                                     op0=ALU.mult, op1=ALU.mult), [gs, rq, g_q], [cqn])
        P.op("act", lambda e, gs=gs: e.activation(out=junk2[:, 384:640], in_=gs[:, 384:640], func=AF.Square, accum_out=sskv[:]), [gs], [junk2, sskv])
        _rstd(P, sskv, rkv, 256)
        P.op("dve", lambda e, gs=gs: e.scalar_tensor_tensor(out=ckvn[:], in0=gs[:, 384:640], scalar=rkv[:], in1=g_kv[:],
                                                     op0=ALU.mult, op1=ALU.mult), [gs, rkv, g_kv], [ckvn])
        xk = gs[:, 640:672].rearrange("p (a d) -> p a d", a=2)
        P.op("dve", lambda e, T=T, xk=xk: e.tensor_tensor(out=ra[:, 0:32].rearrange("p (a d) -> p a d", a=2), in0=xk,
                                                   in1=cos_ml[:, T, :].unsqueeze(1).broadcast_to([128, 2, 16]), op=ALU.mult),
             [gs, cos_ml], [ra])
        P.op("dve", lambda e, T=T, xk=xk: e.tensor_tensor(out=rb[:, 0:32].rearrange("p (a d) -> p a d", a=2), in0=xk,
                                                   in1=sin_ml[:, T, :].unsqueeze(1).broadcast_to([128, 2, 16]), op=ALU.mult),
             [gs, sin_ml], [rb])
        P.op("dve", lambda e: e.tensor_tensor(out=kr96[:, 64:80], in0=ra[:, 0:16], in1=rb[:, 16:32], op=ALU.subtract), [ra, rb], [kr96])
        P.op("dve", lambda e: e.tensor_tensor(out=kr96[:, 80:96], in0=ra[:, 16:32], in1=rb[:, 0:16], op=ALU.add), [ra, rb], [kr96])
        xs_ = gs[:, 672:1056].rearrange("p (h a d) -> p h a d", h=6, a=2)
        P.op("dve", lambda e, T=T, xs_=xs_: e.tensor_tensor(out=ra[:].rearrange("p (h a d) -> p h a d", h=6, a=2), in0=xs_,
                                                   in1=cos_sw[:, T, :].unsqueeze(1).unsqueeze(1).broadcast_to([128, 6, 2, 32]), op=ALU.mult),
             [gs, cos_sw], [ra])
        P.op("dve", lambda e, T=T, xs_=xs_: e.tensor_tensor(out=rb[:].rearrange("p (h a d) -> p h a d", h=6, a=2), in0=xs_,
                                                   in1=sin_sw[:, T, :].unsqueeze(1).unsqueeze(1).broadcast_to([128, 6, 2, 32]), op=ALU.mult),
             [gs, sin_sw], [rb])
        ra4 = ra[:].rearrange("p (h a d) -> p h a d", h=6, a=2)
        rb4 = rb[:].rearrange("p (h a d) -> p h a d", h=6, a=2)
        P.op("dve", lambda e: e.tensor_tensor(out=swqk[:, 0:4, 0:32], in0=ra4[:, 0:4, 0, :], in1=rb4[:, 0:4, 1, :], op=ALU.subtract), [ra, rb], [swqk])
        P.op("dve", lambda e: e.tensor_tensor(out=swqk[:, 0:4, 32:64], in0=ra4[:, 0:4, 1, :], in1=rb4[:, 0:4, 0, :], op=ALU.add), [ra, rb], [swqk])
        P.op("dve", lambda e: e.tensor_tensor(out=krt[:, :, 0:32], in0=ra4[:, 4:6, 0, :], in1=rb4[:, 4:6, 1, :], op=ALU.subtract), [ra, rb], [krt])
        P.op("dve", lambda e: e.tensor_tensor(out=krt[:, :, 32:64], in0=ra4[:, 4:6, 1, :], in1=rb4[:, 4:6, 0, :], op=ALU.add), [ra, rb], [krt])
        P.op("pool", lambda e: e.tensor_copy(out=swqk[:, 4:8, :].rearrange("p (g u) d -> p g u d", u=2),
                                             in_=krt[:].unsqueeze(2).broadcast_to([128, 2, 2, 64])), [krt], [swqk])

    def post_b(T):
        ch, tt = T // 4, T % 4
        s = st[ch % 2]
        nq = naqks[T % 2]
        for cb in range(4):
            _tr(P, bf(6, cb * 128, (cb + 1) * 128), nq[:, cb * 128:(cb + 1) * 128], K.ident[:], [nq, K.ident], [b6])
        for cb in range(3):
            _tr(P, bf(7, cb * 128, (cb + 1) * 128), cqn[:, cb * 128:(cb + 1) * 128], K.ident[:], [cqn, K.ident], [b7])
        for cb in range(2):
            _tr(P, bf(7, (3 + cb) * 128, (4 + cb) * 128), ckvn[:, cb * 128:(cb + 1) * 128], K.ident[:], [ckvn, K.ident], [b7])
        _tr(P, psb[0:96, 7 * 1024 + 5 * 128: 7 * 1024 + 6 * 128], kr96[:, 0:96], K.ident[:], [kr96, K.ident], [b7])
        swf = swqk[:].rearrange("p h d -> p (h d)")
        for cb in range(4):
            _tr(P, bf(6, (4 + cb) * 128, (5 + cb) * 128), swf[:, cb * 128:(cb + 1) * 128], K.ident[:], [swqk, K.ident], [b6])
        tsl = slice(tt * 128, (tt + 1) * 128)
        P.op("act", lambda e, s=s, tsl=tsl: e.activation(out=s.naqk[:, 0:2, tsl], in_=bf(6, 0, 256).rearrange("p (c t) -> p c t", c=2),
                                                         func=AF.Copy, scale=0.125), [b6], [s.naqk])
        P.op("act", lambda e, s=s, tsl=tsl: e.activation(out=s.naqk[:, 2:4, tsl], in_=bf(6, 256, 512).rearrange("p (c t) -> p c t", c=2), func=AF.Copy), [b6], [s.naqk])
        P.op("act", lambda e, s=s, tsl=tsl: e.activation(out=s.swqk[:, 0:2, tsl], in_=bf(6, 512, 768).rearrange("p (c t) -> p c t", c=2),
                                                         func=AF.Copy, scale=0.125), [b6], [s.swqk])
        P.op("act", lambda e, s=s, tsl=tsl: e.activation(out=s.swqk[:, 2:4, tsl], in_=bf(6, 768, 1024).rearrange("p (c t) -> p c t", c=2), func=AF.Copy), [b6], [s.swqk])
        P.op("act", lambda e, s=s, tsl=tsl: e.activation(out=s.cq[:, :, tsl], in_=bf(7, 0, 384).rearrange("p (c t) -> p c t", c=3), func=AF.Copy), [b7], [s.cq])
        P.op("act", lambda e, s=s, tsl=tsl: e.activation(out=s.ckv[:, :, tsl], in_=bf(7, 384, 640).rearrange("p (c t) -> p c t", c=2), func=AF.Copy), [b7], [s.ckv])
        P.op("act", lambda e, s=s, tsl=tsl: e.activation(out=s.kr[64:96, tsl], in_=psb[64:96, 7 * 1024 + 640: 7 * 1024 + 768], func=AF.Copy), [b7], [s.kr])
        if tt == 3:
            cs = slice(ch * 512, (ch + 1) * 512)
            rs_ = slice(ch * 512, (ch + 1) * 512)
            P.dma("sp", lambda e, s=s, cs=cs: e.dma_start(out=C.naqT_d[:, :, cs].rearrange("c p t -> p c t"), in_=s.naqk[:, 0:2, :]), [s.naqk], [C.B["naqT"][ch]])
            P.dma("sp", lambda e, s=s, cs=cs: e.dma_start(out=C.nakT_d[:, :, cs].rearrange("c p t -> p c t"), in_=s.naqk[:, 2:4, :]), [s.naqk], [C.B["nakT"][ch]])
            P.dma("sp", lambda e, s=s, rs_=rs_: e.dma_start(out=C.nav_d[rs_, :].rearrange("(t p) c -> p t c", p=128), in_=s.nav[:].rearrange("p t h d -> p t (h d)")), [s.nav], [C.B["nav"][ch]])
            P.dma("sp", lambda e, s=s, cs=cs: e.dma_start(out=C.cqT_d[:, :, cs].rearrange("c p t -> p c t"), in_=s.cq[:]), [s.cq], [C.B["cqT"][ch]])
            P.dma("sp", lambda e, s=s, cs=cs: e.dma_start(out=C.ckvT_d[:, :, cs].rearrange("c p t -> p c t"), in_=s.ckv[:]), [s.ckv], [C.B["ckvT"][ch]])
            P.dma("sp", lambda e, s=s, cs=cs: e.dma_start(out=C.krT_d[:, cs], in_=s.kr[64:96, :]), [s.kr], [C.B["krT"][ch]])
            P.dma("sp", lambda e, s=s, cs=cs: e.dma_start(out=C.swqT_d[:, :, cs].rearrange("c p t -> p c t"), in_=s.swqk[:, 0:2, :]), [s.swqk], [C.B["swqT"][ch]])
            P.dma("sp", lambda e, s=s, cs=cs: e.dma_start(out=C.swkT_d[:, :, cs].rearrange("c p t -> p c t"), in_=s.swqk[:, 2:4, :]), [s.swqk], [C.B["swkT"][ch]])
            P.dma("sp", lambda e, s=s, rs_=rs_: e.dma_start(out=C.swv_d[rs_, :].rearrange("(t p) c -> p t c", p=128), in_=s.swv[:].rearrange("p t h d -> p t (h d)")), [s.swv], [C.B["swv"][ch]])

    junk2 = P.sb([128, 640], BF16, "junk2")
    fa(0)
    if NTT > 1:
        fa(1)
    fb(0)
    for T in range(NTT):
        if T + 2 < NTT:
            fa(T + 2)
        if T + 1 < NTT:
            fb(T + 1)
        mm(T)
        if T >= 1:
            post_a(T - 1)
        gc(T)
        if T >= 1:
            post_b(T - 1)
    post_a(NTT - 1)
    post_b(NTT - 1)
    P.barrier()
    P.release(mk)


NA_KINDS = [(10, d) for d in (-2, -1, 0, 1, 2)] + [(0, d) for d in (0, 1, 2, 3)] + [(1, d) for d in (-1, 0, 1, 2)] + \
           [(30, d) for d in (-2, -1, 0, 1)] + [(31, d) for d in (-3, -2, -1, 0)]


def na_blocks(j):
    if j == 0:
        return [(d, 5 + d) for d in range(4)]
    if j == 1:
        return [(1 + d, 9 + (d + 1)) for d in (-1, 0, 1, 2)]
    if j == 30:
        return [(30 + d, 13 + (d + 2)) for d in (-2, -1, 0, 1)]
    if j == 31:
        return [(31 + d, 17 + (d + 3)) for d in (-3, -2, -1, 0)]
    return [(j + d, d + 2) for d in (-2, -1, 0, 1, 2)]


def sw_blocks(j):
    out = []
    if j > 0:
        out.append((j - 1, 0))
    out.append((j, None))
    if j < NT - 1:
        out.append((j + 1, 1))
    return out


def phase_local(P, C, K, l, kind):
    mk = P.mark()
    if kind == "na":
        pre_w_out(P, C, l)
    ps, psb, bk = C.ps, C.psb, C.bk
    na = kind == "na"
    H = 4
    HV = 4 if na else 2
    qT = P.sb([128, 2, S], BF16, "qT")
    kT = P.sb([128, 2, S], BF16, "kT")
    v = P.sb([128, NT, HV * 65], BF16, "v")
    qd, kd, vd = (C.naqT_d, C.nakT_d, C.nav_d) if na else (C.swqT_d, C.swkT_d, C.swv_d)
    qB, kB, vB = (C.B["naqT"], C.B["nakT"], C.B["nav"]) if na else (C.B["swqT"], C.B["swkT"], C.B["swv"])
    for c in range(2):
        P.dma("sp", lambda e, c=c: e.dma_start(out=qT[:, c, :], in_=qd[c]), qB, [qT])
        P.dma("sp", lambda e, c=c: e.dma_start(out=kT[:, c, :], in_=kd[c]), kB, [kT])
    P.dma("sp", lambda e: e.dma_start(out=v[:], in_=vd.rearrange("(t p) c -> p t c", p=128)), vB, [v])
    if na:
        nbk = P.sb([128, 4, 21, 128], BF16, "nbk")
        msk = P.sb([128, 21, 128], F32, "msk")
        stg = P.sb([128, 21, 128], F32, "stg")
        P.dma("sp", lambda e: e.dma_start(out=msk[:], in_=C.namask.rearrange("k p q -> p k q")), [], [msk])
        for h in range(4):
            P.dma("sp", lambda e, h=h: e.dma_start(out=stg[:], in_=C.nab[l, h].rearrange("k p q -> p k q")), [], [stg])
            P.op("dve", lambda e, h=h: e.tensor_tensor(out=stg[:], in0=stg[:], in1=msk[:], op=ALU.add), [stg, msk], [stg])
            P.op("act", lambda e, h=h: e.activation(out=nbk[:, h], in_=stg[:], func=AF.Exp), [stg], [nbk])
        biasB = nbk
    else:
        swm = P.sb([128, 3, 128], BF16, "swm")
        swf32 = P.sb([128, 2, 128], F32, "swf32")
        P.dma("sp", lambda e: e.dma_start(out=swf32[:], in_=C.swmask.rearrange("k p q -> p k q")), [], [swf32])
        P.op("dve", lambda e: e.memset(swm[:], 1.0), [], [swm])
        P.op("act", lambda e: e.activation(out=swm[:, 0, :], in_=swf32[:, 0, :], func=AF.Exp), [swf32], [swm])
        P.op("act", lambda e: e.activation(out=swm[:, 2, :], in_=swf32[:, 1, :], func=AF.Exp), [swf32], [swm])
        esink = P.sb([128, 4], F32, "esink")
        P.dma("sp", lambda e: e.dma_start(out=esink[:], in_=C.swa_sink[l].partition_broadcast(128)), [], [esink])
        P.op("act", lambda e: e.activation(out=esink[:], in_=esink[:], func=AF.Exp), [esink], [esink])
        biasB = swm
    pts = [P.sb([128, 640], BF16, "pt") for _ in range(3)]
    den = P.sb([128, 4], F32, "den")
    otok = [P.sb([128, 4, 64], BF16, "otok") for _ in range(2)]
    sto = [P.sb([128, 2, 512], BF16, "sto") for _ in range(2)]
    spair = [Buf(None, "sp01"), Buf(None, "sp23")]
    ob = [bk[4], bk[5]]
    cbase = 0 if na else 6
    it = 0
    def s_stage(j, h, it):
        blocks = na_blocks(j) if na else sw_blocks(j)
        nb = len(blocks)
        sp_ = spair[it % 2]
        sbase = (it % 2) * 1024
        pt = pts[it % 3]
        pr = slice((h % 2) * 64, (h % 2) * 64 + 64)
        for bi, (kt, bkind) in enumerate(blocks):
            o_ap = ps[:, sbase + bi * 128: sbase + (bi + 1) * 128]
            _mm(P, o_ap, kT[pr, h // 2, kt * 128:(kt + 1) * 128], qT[pr, h // 2, j * 128:(j + 1) * 128], True, True, [kT, qT], [sp_])
        P.op("act", lambda e, pt=pt, sbase=sbase, nb=nb: e.activation(out=pt[:, 0:nb * 128], in_=ps[:, sbase: sbase + nb * 128], func=AF.Exp), [sp_], [pt])
        if na:
            k0 = blocks[0][1]
            eb = nbk[:, h, k0:k0 + nb, :].rearrange("p k q -> p (k q)")
        else:
            b0 = 1 if j == 0 else 0
            eb = swm[:, b0:b0 + nb, :].rearrange("p k q -> p (k q)")
        P.op("dve", lambda e, pt=pt, nb=nb, eb=eb: e.tensor_tensor(out=pt[:, 0:nb * 128], in0=pt[:, 0:nb * 128], in1=eb, op=ALU.mult), [pt, biasB], [pt])

    def pv_stage(j, h, it):
        blocks = na_blocks(j) if na else sw_blocks(j)
        nb = len(blocks)
        pt = pts[it % 3]
        jo = j % 2
        hv = h if na else h // 2
        for bi, (kt, bkind) in enumerate(blocks):
            _mm(P, ps[:, (4 + jo) * 512 + h * 65: (4 + jo) * 512 + (h + 1) * 65], pt[:, bi * 128:(bi + 1) * 128],
                v[:, kt, hv * 65:(hv + 1) * 65], bi == 0, bi == nb - 1, [pt, v], [ob[jo]])

    def fin_stage(j):
        ch, tt = j // 4, j % 4
        jo = j % 2
        ov = ps[:, (4 + jo) * 512: (4 + jo) * 512 + 260].rearrange("p (h d) -> p h d", h=4)
        dn = dens[jo]
        if na:
            P.op("dve", lambda e, ov=ov, dn=dn: e.reciprocal(out=dn[:], in_=ov[:, :, 64]), [ob[jo]], [dn])
        else:
            P.op("dve", lambda e, ov=ov, dn=dn: e.tensor_tensor(out=dn[:], in0=ov[:, :, 64], in1=esink[:], op=ALU.add), [ob[jo], esink], [dn])
            P.op("dve", lambda e, dn=dn: e.reciprocal(out=dn[:], in_=dn[:]), [dn], [dn])
        ot = otok[jo]
        P.op("dve", lambda e, ov=ov, ot=ot, dn=dn: e.tensor_tensor(out=ot[:], in0=ov[:, :, 0:64], in1=dn[:].unsqueeze(2).broadcast_to([128, 4, 64]), op=ALU.mult),
             [ob[jo], dn], [ot])
        otf = ot[:].rearrange("p h d -> p (h d)")
        tb = 6 + jo
        for cb in range(2):
            _tr(P, psb[:, tb * 1024 + cb * 128: tb * 1024 + (cb + 1) * 128], otf[:, cb * 128:(cb + 1) * 128], K.ident[:], [ot, K.ident], [bk[tb]])
        so = sto[ch % 2]
        P.op("act", lambda e, so=so, tt=tt, tb=tb: e.activation(out=so[:, :, tt * 128:(tt + 1) * 128],
                                                         in_=psb[:, tb * 1024: tb * 1024 + 256].rearrange("p (c t) -> p c t", c=2), func=AF.Copy), [bk[tb]], [so])
        if tt == 3:
            P.dma("sp", lambda e, so=so, ch=ch: e.dma_start(out=C.oT_d[cbase:cbase + 2, :, ch * 512:(ch + 1) * 512].rearrange("c p t -> p c t"), in_=so[:]),
                  [so], [C.B["oT_" + kind][ch]])

    dens = [den, P.sb([128, 4], F32, "den2")]
    items = [(j, h) for j in range(NT) for h in range(H)]
    s_stage(items[0][0], items[0][1], 0)
    for n, (j, h) in enumerate(items):
        if n + 1 < len(items):
            s_stage(items[n + 1][0], items[n + 1][1], n + 1)
        pv_stage(j, h, n)
        if h == H - 1:
            fin_stage(j)
    P.barrier()
    P.release(mk)


def phase_mla(P, C, K, l):
    mk = P.mark()
    ps, psb, bk = C.ps, C.psb, C.bk
    cqT = P.sb([128, 3, S], BF16, "cqT")
    KT = P.sb([128, 8, S], BF16, "KT")
    Vv = P.sb([128, NT, 8, 65], BF16, "Vv")
    w_q = P.sb([128, 3, 768], BF16, "w_q")
    w_qr = P.sb([128, 3, 768], BF16, "w_qr")
    for c in range(3):
        P.dma("sp", lambda e, c=c: e.dma_start(out=cqT[:, c, :], in_=C.cqT_d[c]), C.B["cqT"], [cqT])
    P.dma("pool", lambda e: e.dma_start(out=w_q[:], in_=C.mla_w_q_up[l].rearrange("(c p) n -> p c n", p=128)), [], [w_q])
    P.op("pool", lambda e: e.memset(w_qr[:], 0.0), [], [w_qr])
    wq4 = w_q[:].rearrange("p c (h d) -> p c h d", h=8)
    wr4 = w_qr[:].rearrange("p c (h d) -> p c h d", h=8)
    for c in range(3):
        P.op("act", lambda e, c=c: e.activation(out=wr4[:, c, :, 64:80], in_=wq4[:, c, :, 80:96], func=AF.Copy, scale=-1.0), [w_q], [w_qr])
        P.op("dve", lambda e, c=c: e.tensor_copy(out=wr4[:, c, :, 80:96], in_=wq4[:, c, :, 64:80]), [w_q], [w_qr])
    P.op("pool", lambda e: e.memset(Vv[:], 1.0), [], [Vv])
    for h in range(8):
        P.dma("sp", lambda e, h=h: e.dma_start(out=KT[64:96, h, :], in_=C.krT_d), C.B["krT"], [KT])
    mk2 = P.mark()
    ckvT = P.sb([128, 2, S], BF16, "ckvT")
    w_kv = P.sb([128, 2, 1024], BF16, "w_kv")
    for c in range(2):
        P.dma("sp", lambda e, c=c: e.dma_start(out=ckvT[:, c, :], in_=C.ckvT_d[c]), C.B["ckvT"], [ckvT])
    P.dma("pool", lambda e: e.dma_start(out=w_kv[:], in_=C.mla_w_kv_up[l].rearrange("(c p) n -> p c n", p=128)), [], [w_kv])
    wkv4 = w_kv[:].rearrange("p c (h a d) -> p c h a d", h=8, a=2)
    it = 0
    for c in range(NCH):
        for h in range(8):
            b = it % 4
            it += 1
            for kc in range(2):
                _mm(P, ps[0:64, b * 512:(b + 1) * 512], w_kv[:, kc, h * 128:h * 128 + 64], ckvT[:, kc, c * 512:(c + 1) * 512], kc == 0, kc == 1, [w_kv, ckvT], [bk[b]])
            if it % 2:
                P.op("act", lambda e, b=b, h=h, c=c: e.activation(out=KT[0:64, h, c * 512:(c + 1) * 512], in_=ps[0:64, b * 512:(b + 1) * 512], func=AF.Copy), [bk[b]], [KT])
            else:
                P.op("dve", lambda e, b=b, h=h, c=c: e.tensor_copy(out=KT[0:64, h, c * 512:(c + 1) * 512], in_=ps[0:64, b * 512:(b + 1) * 512]), [bk[b]], [KT])
    for T in range(NT):
        b = 4 + T % 2
        for kc in range(2):
            _mm(P, ps[:, b * 512:(b + 1) * 512], ckvT[:, kc, T * 128:(T + 1) * 128], wkv4[:, kc, :, 1, :], kc == 0, kc == 1, [w_kv, ckvT], [bk[b]])
        eng = "act" if T % 2 else "dve"
        if eng == "act":
            P.op("act", lambda e, b=b, T=T: e.activation(out=Vv[:, T, :, 0:64], in_=ps[:, b * 512:(b + 1) * 512].rearrange("p (h d) -> p h d", h=8), func=AF.Copy), [bk[b]], [Vv])
        else:
            P.op("dve", lambda e, b=b, T=T: e.tensor_copy(out=Vv[:, T, :, 0:64], in_=ps[:, b * 512:(b + 1) * 512].rearrange("p (h d) -> p h d", h=8)), [bk[b]], [Vv])
    P.barrier()
    P.release(mk2)
    cst = [P.sb([128, 512], F32, "cst") for _ in range(2)]
    snt = [P.sb([128, 512], F32, "snt") for _ in range(2)]
    QT = [P.sb([128, 512], BF16, "QT") for _ in range(2)]
    t1 = P.sb([128, 512], F32, "t1")
    t2 = P.sb([128, 512], F32, "t2")
    PT = [P.sb([128, 512], BF16, "PT") for _ in range(3)]
    rrow = P.sb([128, 512], F32, "rrow")
    bcs = P.sb([64, 512], F32, "bcs")
    sto = [P.sb([64, 512], BF16, "sto") for _ in range(2)]
    scale = float(96 ** -0.5)
    si = 0
    qi = 0
    def load_cs(c):
        cs = slice(c * 512, (c + 1) * 512)
        ct, sn = cst[c % 2], snt[c % 2]
        P.dma("sp", lambda e, ct=ct, cs=cs: e.dma_start(out=ct[64:96, :], in_=C.cosT_mla[:, cs]), [], [ct])
        P.dma("sp", lambda e, sn=sn, cs=cs: e.dma_start(out=sn[64:96, :], in_=C.sinT_mla[:, cs]), [], [sn])

    def qproj(i):
        c, h = i // 8, i % 8
        cs = slice(c * 512, (c + 1) * 512)
        ct, sn = cst[c % 2], snt[c % 2]
        q = QT[i % 2]
        for kc in range(3):
            _mm(P, ps[0:96, 5 * 512:6 * 512], w_q[:, kc, h * 96:(h + 1) * 96], cqT[:, kc, cs], kc == 0, kc == 2, [w_q, cqT], [bk[5]])
        for kc in range(3):
            _mm(P, ps[0:96, 6 * 512:7 * 512], w_qr[:, kc, h * 96:(h + 1) * 96], cqT[:, kc, cs], kc == 0, kc == 2, [w_qr, cqT], [bk[6]])
        P.op("dve", lambda e, q=q: e.tensor_copy(out=q[0:64, :], in_=ps[0:64, 5 * 512:6 * 512]), [bk[5]], [q])
        P.op("dve", lambda e, ct=ct: e.tensor_tensor(out=t1[64:96, :], in0=ps[64:96, 5 * 512:6 * 512], in1=ct[64:96, :], op=ALU.mult), [bk[5], ct], [t1])
        P.op("dve", lambda e, sn=sn: e.tensor_tensor(out=t2[64:96, :], in0=ps[64:96, 6 * 512:7 * 512], in1=sn[64:96, :], op=ALU.mult), [bk[6], sn], [t2])
        P.op("dve", lambda e, q=q: e.tensor_tensor(out=q[64:96, :], in0=t1[64:96, :], in1=t2[64:96, :], op=ALU.add), [t1, t2], [q])

    def tail(i):
        c, h = i // 8, i % 8
        cs = slice(c * 512, (c + 1) * 512)
        ob = 3 + i % 2
        so = sto[i % 2]
        P.op("dve", lambda e, ob=ob: e.reciprocal(out=rrow[64:65, :], in_=ps[64:65, ob * 512:(ob + 1) * 512]), [bk[ob]], [rrow])
        _mm(P, ps[0:64, 7 * 512:8 * 512], K.onesf[64:65, 0:64], rrow[64:65, :], True, True, [K.onesf, rrow], [bk[7]])
        P.op("dve", lambda e: e.tensor_copy(out=bcs[:], in_=ps[0:64, 7 * 512:8 * 512]), [bk[7]], [bcs])
        P.op("dve", lambda e, so=so, ob=ob: e.tensor_tensor(out=so[:], in0=ps[0:64, ob * 512:(ob + 1) * 512], in1=bcs[:], op=ALU.mult), [bk[ob], bcs], [so])
        P.dma("sp", lambda e, so=so, h=h, cs=cs: e.dma_start(out=C.oT_d[2 + h // 2, (h % 2) * 64:(h % 2) * 64 + 64, cs], in_=so[:]), [so], [C.B["oT_mla"][c]])

    NI = NCH * 8
    load_cs(0)
    qproj(0)
    for i in range(NI):
        c, h = i // 8, i % 8
        q = QT[i % 2]
        ob = 3 + i % 2
        sbs = []
        for step in range(NT + 2):
            if step == 3 and h == 0 and c + 1 < NCH:
                load_cs(c + 1)
            if step == 2 and i >= 1:
                tail(i - 1)
            if step == 16 and i + 1 < NI:
                qproj(i + 1)
            if step < NT:
                kt = step
                sb_ = si % 3
                pt = PT[si % 3]
                si += 1
                sbs.append((sb_, pt))
                _mm(P, ps[:, sb_ * 512:(sb_ + 1) * 512], KT[0:96, h, kt * 128:(kt + 1) * 128], q[0:96, :], True, True, [KT, q], [bk[sb_]])
                P.op("act", lambda e, pt=pt, sb_=sb_: e.activation(out=pt[:], in_=ps[:, sb_ * 512:(sb_ + 1) * 512], func=AF.Exp, scale=scale), [bk[sb_]], [pt])
            if step >= 2:
                kt = step - 2
                sb_, pt = sbs[kt]
                _mm(P, ps[0:65, ob * 512:(ob + 1) * 512], Vv[:, kt, h, :], pt[:], kt == 0, kt == NT - 1, [Vv, pt], [bk[ob]])
    tail(NI - 1)
    P.barrier()
    P.release(mk)


def phase_O(P, C, K, l):
    mk = P.mark()
    ps, psb, bk = C.ps, C.psb, C.bk
    w_out = C.v_w_out
    w_r = C.v_w_r
    g_f = P.sb([128, 1024], F32, "g_f")
    P.dma("sp", lambda e: e.dma_start(out=g_f[:], in_=C.ffn_norm[l].partition_broadcast(128)), [], [g_f])
    oTs = [P.sb([128, 8, 512], BF16, "oTs") for _ in range(2)]
    hts = [P.sb([128, 1024], F32, "ht") for _ in range(2)]
    h2s = [P.sb([128, 1024], F32, "h2") for _ in range(2)]
    xns = [P.sb([128, 1024], BF16, "xn") for _ in range(2)]
    xTs = [P.sb([128, 8, 128], BF16, "xT") for _ in range(2)]
    junk = P.sb([128, 1024], BF16, "junk")
    ss = [P.sb([128, 1], F32, "ss") for _ in range(2)]
    rs = [P.sb([128, 1], F32, "rs") for _ in range(2)]
    exs = [P.sb([128, 16], F32, "ex") for _ in range(2)]
    sumes = [P.sb([128, 1], F32, "sume") for _ in range(2)]
    src = C.x if l == 0 else C.h_d
    srcB = C.B["x"] if l == 0 else C.B["h"]
    oTB = lambda ch: [C.B["oT_na"][ch], C.B["oT_mla"][ch], C.B["oT_sw"][ch]]
    def front(T):
        ch, tt = T // 4, T % 4
        oT = oTs[ch % 2]
        if tt == 0:
            P.dma("sp", lambda e, oT=oT, ch=ch: e.dma_start(out=oT[:], in_=C.oT_d[:, :, ch * 512:(ch + 1) * 512].rearrange("c p t -> p c t")), oTB(ch), [oT])
        ht, h2 = hts[T % 2], h2s[T % 2]
        P.dma("sp", lambda e, ht=ht, T=T: e.dma_start(out=ht[:], in_=src[T * 128:(T + 1) * 128, :]), [srcB[T]], [ht])
        for half in range(2):
            for k in range(8):
                _mm(P, ps[:, half * 512:(half + 1) * 512], oT[:, k, tt * 128:(tt + 1) * 128], w_out[:, k, half * 512:(half + 1) * 512], k == 0, k == 7, [oT, w_out], [bk[half]])
            P.op("dve", lambda e, half=half, ht=ht, h2=h2: e.tensor_tensor(out=h2[:, half * 512:(half + 1) * 512], in0=ps[:, half * 512:(half + 1) * 512],
                                                                          in1=ht[:, half * 512:(half + 1) * 512], op=ALU.add), [bk[half], ht], [h2])
        P.dma("sp", lambda e, h2=h2, T=T: e.dma_start(out=C.h_d[T * 128:(T + 1) * 128, :], in_=h2[:]), [h2], [C.B["h"][T]])

    def back(T):
        h2, xn, xT = h2s[T % 2], xns[T % 2], xTs[T % 2]
        P.op("act", lambda e, h2=h2, T=T: e.activation(out=junk[:], in_=h2[:], func=AF.Square, accum_out=ss[T % 2][:]), [h2], [junk, ss[T % 2]])
        _rstd(P, ss[T % 2], rs[T % 2], 1024)
        P.op("dve", lambda e, h2=h2, xn=xn, T=T: e.scalar_tensor_tensor(out=xn[:], in0=h2[:], scalar=rs[T % 2][:], in1=g_f[:], op0=ALU.mult, op1=ALU.mult),
             [h2, rs[T % 2], g_f], [xn])
        P.dma("sp", lambda e, xn=xn, T=T: e.dma_start(out=C.hnb_d[T * 128:(T + 1) * 128, :], in_=xn[:]), [xn], [C.B["hnb"][T]])
        tb = 5 + T % 2
        for c in range(8):
            _tr(P, psb[:, tb * 1024 + c * 128: tb * 1024 + (c + 1) * 128], xn[:, c * 128:(c + 1) * 128], K.ident[:], [xn, K.ident], [bk[tb]])
        P.op("act", lambda e, xT=xT, tb=tb: e.activation(out=xT[:].rearrange("p c t -> p (c t)"), in_=psb[:, tb * 1024: (tb + 1) * 1024], func=AF.Copy), [bk[tb]], [xT])
        rb_ = 2 + T % 2
        for k in range(8):
            _mm(P, ps[:, rb_ * 512: rb_ * 512 + 16], xT[:, k, :], w_r[:, k, :], k == 0, k == 7, [xT, w_r], [bk[rb_]])
        ex, sume = exs[T % 2], sumes[T % 2]
        P.op("act", lambda e, ex=ex, sume=sume, rb_=rb_: e.activation(out=ex[:], in_=ps[:, rb_ * 512: rb_ * 512 + 16], func=AF.Exp, accum_out=sume[:]), [bk[rb_]], [ex, sume])
        P.op("dve", lambda e, sume=sume: e.reciprocal(out=sume[:], in_=sume[:]), [sume], [sume])
        P.op("dve", lambda e, T=T, ex=ex, sume=sume: e.tensor_scalar(out=K.aff[:, T, :], in0=ex[:], scalar1=sume[:], scalar2=None, op0=ALU.mult), [ex, sume], [K.aff])

    for T in range(NT + 1):
        if T < NT:
            front(T)
        if T >= 1:
            back(T - 1)
    P.barrier()
    P.release(mk)


def phase_R(P, C, K, l):
    mk = P.mark()
    pre_exp0(P, C, l)
    ps, psb, bk = C.ps, C.psb, C.bk
    lo = P.sb([128, 16], F32, "lo")
    mid = P.sb([128, 16], F32, "mid")
    cnt = P.sb([128, 16], F32, "cnt")
    m_ = P.sb([128, 16], F32, "m_")
    cmp_ = [P.sb([128, 32, 16], BF16, "cmp") for _ in range(2)]
    aff = K.aff
    P.op("dve", lambda e: e.memset(lo[:], 0.0), [], [lo])
    NIT = 26
    for it in range(NIT):
        w = 2.0 ** -(it + 1)
        cm = cmp_[it % 2]
        b = it % 2
        P.op("dve", lambda e, w=w: e.tensor_scalar(out=mid[:], in0=lo[:], scalar1=w, scalar2=None, op0=ALU.add), [lo], [mid])
        P.op("dve", lambda e, cm=cm: e.tensor_tensor(out=cm[:], in0=aff[:], in1=mid[:].unsqueeze(1).broadcast_to([128, 32, 16]), op=ALU.is_gt), [aff, mid], [cm])
        _mm(P, ps[:, b * 512:(b + 1) * 512], K.ones[:], cm[:].rearrange("p i e -> p (i e)"), True, True, [K.ones, cm], [bk[b]])
        P.op("dve", lambda e, b=b: e.tensor_reduce(out=cnt[:], in_=ps[:, b * 512:(b + 1) * 512].rearrange("p (i e) -> p e i", e=16), axis=AX.X, op=ALU.add), [bk[b]], [cnt])
        P.op("dve", lambda e: e.tensor_scalar(out=m_[:], in0=cnt[:], scalar1=511.5, scalar2=None, op0=ALU.is_gt), [cnt], [m_])
        P.op("dve", lambda e, w=w: e.scalar_tensor_tensor(out=lo[:], in0=m_[:], scalar=w, in1=lo[:], op0=ALU.mult, op1=ALU.add), [m_, lo], [lo])
    mask = P.sb([128, 32, 16], F32, "mask")
    maskb = P.sb([128, 32, 16], BF16, "maskb")
    gm = P.sb([128, 32, 16], F32, "gm")
    P.op("dve", lambda e: e.tensor_tensor(out=mask[:], in0=aff[:], in1=lo[:].unsqueeze(1).broadcast_to([128, 32, 16]), op=ALU.is_gt), [aff, lo], [mask])
    P.op("dve", lambda e: e.tensor_copy(out=maskb[:], in_=mask[:]), [mask], [maskb])
    P.op("dve", lambda e: e.tensor_tensor(out=gm[:], in0=aff[:], in1=mask[:], op=ALU.mult), [aff, mask], [gm])
    mflat = maskb[:].rearrange("p i e -> p (i e)")
    _mm(P, ps[:, 0:512], K.ones[:], mflat, True, True, [K.ones, maskb], [bk[0]])
    _mm(P, ps[:, 512:1024], K.L[:], mflat, True, True, [K.L, maskb], [bk[1]])
    cnt_ei = P.sb([128, 16, 32], F32, "cnt_ei")
    cum = P.sb([128, 16, 32], F32, "cum")
    rst = P.sb([128, 16, 32], F32, "rst")
    pos = P.sb([128, 32, 16], F32, "pos")
    tmp = P.sb([128, 32, 16], F32, "tmp")
    P.op("dve", lambda e: e.tensor_copy(out=cnt_ei[:], in_=ps[:, 0:512].rearrange("p (i e) -> p e i", e=16)), [bk[0]], [cnt_ei])
    P.op("pool", lambda e: e.memset(rst[:], 1.0), [], [rst])
    P.op("pool", lambda e: e.memset(rst[:, :, 0:1], 0.0), [rst], [rst])
    P.op("dve", lambda e: e.tensor_tensor_scan(out=cum[:].rearrange("p e i -> p (e i)"), data0=rst[:].rearrange("p e i -> p (e i)"),
                                               data1=cnt_ei[:].rearrange("p e i -> p (e i)"), initial=0.0, op0=ALU.mult, op1=ALU.add), [rst, cnt_ei], [cum])
    P.op("dve", lambda e: e.tensor_tensor(out=cum[:], in0=cum[:], in1=cnt_ei[:], op=ALU.subtract), [cum, cnt_ei], [cum])
    P.op("dve", lambda e: e.tensor_tensor(out=pos[:], in0=ps[:, 512:1024].rearrange("p (i e) -> p i e", e=16), in1=cum[:].rearrange("p e i -> p i e"), op=ALU.add),
         [bk[1], cum], [pos])
    P.op("dve", lambda e: e.tensor_tensor(out=pos[:], in0=pos[:], in1=mask[:], op=ALU.mult), [pos, mask], [pos])
    P.op("dve", lambda e: e.tensor_scalar(out=tmp[:], in0=mask[:], scalar1=-1.0, scalar2=None, op0=ALU.add), [mask], [tmp])
    P.op("dve", lambda e: e.tensor_tensor(out=pos[:], in0=pos[:], in1=tmp[:], op=ALU.add), [pos, tmp], [pos])
    vals = P.sb([128, 32, 16, 4], BF16, "vals")
    ghi = P.sb([128, 32, 16], BF16, "ghi")
    icol = P.sb([128, 32], F32, "icol")
    P.op("dve", lambda e: e.tensor_copy(out=icol[:], in_=K.iota[:, 0:32]), [K.iota], [icol])
    P.op("dve", lambda e: e.tensor_copy(out=vals[:, :, :, 0], in_=K.pcol[:].unsqueeze(2).broadcast_to([128, 32, 16])), [K.pcol], [vals])
    P.op("dve", lambda e: e.tensor_copy(out=vals[:, :, :, 1], in_=icol[:].unsqueeze(2).broadcast_to([128, 32, 16])), [icol], [vals])
    P.op("dve", lambda e: e.tensor_copy(out=ghi[:], in_=gm[:]), [gm], [ghi])
    P.op("dve", lambda e: e.tensor_copy(out=vals[:, :, :, 2], in_=ghi[:]), [ghi], [vals])
    P.op("dve", lambda e: e.tensor_tensor(out=vals[:, :, :, 3], in0=gm[:], in1=ghi[:], op=ALU.subtract), [gm, ghi], [vals])
    Pt = [P.sb([128, 512], BF16, "Pt") for _ in range(4)]
    rsb = [P.sb([4, 512], F32, "rsb") for _ in range(2)]
    tvs = [P.sb([128, 16], F32, "tvs") for _ in range(2)]
    pi = 0
    for ex_ in range(16):
        rb_ = 2 + ex_ % 2
        for i in range(NT):
            pt = Pt[pi % 4]
            eng = "dve"
            pi += 1
            P.op(eng, lambda e, pt=pt, i=i, ex_=ex_: e.tensor_scalar(out=pt[:], in0=K.iota16[:], scalar1=pos[:, i, ex_:ex_ + 1], scalar2=None, op0=ALU.is_equal), [K.iota16, pos], [pt])
            _mm(P, ps[0:4, rb_ * 512:(rb_ + 1) * 512], vals[:, i, ex_, :], pt[:], i == 0, i == NT - 1, [vals, pt], [bk[rb_]])
        r_ = rsb[ex_ % 2]
        P.op("act", lambda e, r_=r_, rb_=rb_: e.activation(out=r_[:], in_=ps[0:4, rb_ * 512:(rb_ + 1) * 512], func=AF.Copy), [bk[rb_]], [r_])
        tb = 4 + ex_ % 2
        for s_ in range(4):
            _tr(P, ps[:, tb * 512 + s_ * 4: tb * 512 + s_ * 4 + 4], r_[:, s_ * 128:(s_ + 1) * 128], K.identf[0:4, 0:4], [r_, K.identf], [bk[tb]])
        tvb = tvs[ex_ % 2]
        P.op("act", lambda e, tvb=tvb, tb=tb: e.activation(out=tvb[:], in_=ps[:, tb * 512: tb * 512 + 16], func=AF.Copy), [bk[tb]], [tvb])
        tv = tvb[:].rearrange("p (s f) -> p s f", f=4)
        P.op("dve", lambda e, tv=tv, ex_=ex_: e.scalar_tensor_tensor(out=K.idx[:, ex_, :], in0=tv[:, :, 1], scalar=128.0, in1=tv[:, :, 0], op0=ALU.mult, op1=ALU.add), [tvb], [K.idx])
        P.op("dve", lambda e, tv=tv, ex_=ex_: e.tensor_tensor(out=K.gsl[:, ex_, :], in0=tv[:, :, 2], in1=tv[:, :, 3], op=ALU.add), [tvb], [K.gsl])
    P.barrier()
    P.release(mk)


def phase_E(P, C, K, l):
    mk = P.mark()
    ps, psb, bk = C.ps, C.psb, C.bk
    wg = [P.sb([128, 8, 512], BF16, "wg") for _ in range(2)]
    wu = [P.sb([128, 8, 512], BF16, "wu") for _ in range(2)]
    wd = [P.sb([128, 4, 1024], BF16, "wd") for _ in range(2)]
    xs = [[P.sb([128, 1024], BF16, "xs") for _ in range(4)] for _ in range(2)]
    xsT = P.sb([128, 8, 512], BF16, "xsT")
    sa = [P.sb([128, 512], F32, "sa") for _ in range(2)]
    hT = P.sb([128, 4, 512], BF16, "hT")
    ys = [P.sb([128, 1024], F32, "y") for _ in range(2)]
    hB = C.B["h"]
    hnB = C.B["hnb"]
    hsc = [[Buf(None, "hsc%d_%d" % (a, b)) for b in range(4)] for a in range(2)]

    def loads(ex_):
        o = ex_ % 2
        if ex_ > 0:
            P.dma("pool", lambda e: e.dma_start(out=wg[o][:], in_=C.w_gate_e[l, ex_].rearrange("(c p) n -> p c n", p=128)), [], [wg[o]])
            P.dma("pool", lambda e: e.dma_start(out=wu[o][:], in_=C.w_up_e[l, ex_].rearrange("(c p) n -> p c n", p=128)), [], [wu[o]])
            P.dma("pool", lambda e: e.dma_start(out=wd[o][:], in_=C.w_down_e[l, ex_].rearrange("(c p) n -> p c n", p=128)), [], [wd[o]])
        for s_ in range(4):
            P.dma("pool", lambda e, s_=s_: e.indirect_dma_start(out=xs[o][s_][:], out_offset=None, in_=C.hnb_d,
                                                               in_offset=bass.IndirectOffsetOnAxis(ap=K.idx[:, ex_, s_:s_ + 1], axis=0)),
                  [K.idx] + hnB, [xs[o][s_]])
    loads(0)
    yi = 0
    for ex_ in range(16):
        o = ex_ % 2
        if ex_ + 1 < 16:
            loads(ex_ + 1)
        if ex_ == 1:
            pre_ple(P, C, l)
        wg_e, wu_e, wd_e = (C.v_wg0, C.v_wu0, C.v_wd0) if ex_ == 0 else (wg[o], wu[o], wd[o])
        for s_ in range(4):
            tb = 6 + s_ % 2
            for c in range(8):
                _tr(P, psb[:, tb * 1024 + c * 128: tb * 1024 + (c + 1) * 128], xs[o][s_][:, c * 128:(c + 1) * 128], K.ident[:], [xs[o][s_], K.ident], [bk[tb]])
            P.op("act", lambda e, s_=s_, tb=tb: e.activation(out=xsT[:, :, s_ * 128:(s_ + 1) * 128], in_=psb[:, tb * 1024:(tb + 1) * 1024].rearrange("p (c t) -> p c t", c=8), func=AF.Copy), [bk[tb]], [xsT])
        for f in range(4):
            ba, bu = (f % 2) * 2, (f % 2) * 2 + 1
            for k in range(8):
                _mm(P, ps[:, ba * 512:(ba + 1) * 512], wg_e[:, k, f * 128:(f + 1) * 128], xsT[:, k, :], k == 0, k == 7, [wg_e, xsT], [bk[ba]])
            for k in range(8):
                _mm(P, ps[:, bu * 512:(bu + 1) * 512], wu_e[:, k, f * 128:(f + 1) * 128], xsT[:, k, :], k == 0, k == 7, [wu_e, xsT], [bk[bu]])
            s1 = sa[f % 2]
            P.op("act", lambda e, s1=s1, ba=ba: e.activation(out=s1[:], in_=ps[:, ba * 512:(ba + 1) * 512], func=AF.Silu), [bk[ba]], [s1])
            P.op("dve", lambda e, s1=s1, bu=bu, f=f: e.tensor_tensor(out=hT[:, f, :], in0=ps[:, bu * 512:(bu + 1) * 512], in1=s1[:], op=ALU.mult), [bk[bu], s1], [hT])
        for s_ in range(4):
            y = ys[yi % 2]
            yi += 1
            for half in range(2):
                bb = 4 + half
                for f in range(4):
                    _mm(P, ps[:, bb * 512:(bb + 1) * 512], hT[:, f, s_ * 128:(s_ + 1) * 128], wd_e[:, f, half * 512:(half + 1) * 512], f == 0, f == 3, [hT, wd_e], [bk[bb]])
                if half:
                    P.op("act", lambda e, y=y, bb=bb, s_=s_, ex_=ex_: e.activation(out=y[:, 512:1024], in_=ps[:, bb * 512:(bb + 1) * 512], func=AF.Copy, scale=K.gsl[:, ex_, s_:s_ + 1]), [bk[bb], K.gsl], [y])
                else:
                    P.op("dve", lambda e, y=y, bb=bb, s_=s_, ex_=ex_: e.tensor_scalar(out=y[:, 0:512], in0=ps[:, bb * 512:(bb + 1) * 512], scalar1=K.gsl[:, ex_, s_:s_ + 1], scalar2=None, op0=ALU.mult), [bk[bb], K.gsl], [y])
            P.dma("pool", lambda e, y=y, s_=s_, ex_=ex_: e.indirect_dma_start(out=C.h_d, out_offset=bass.IndirectOffsetOnAxis(ap=K.idx[:, ex_, s_:s_ + 1], axis=0),
                                                                    in_=y[:], in_offset=None, compute_op=ALU.add),
                  [y, K.idx] + (hsc[(ex_ + 1) % 2] if ex_ > 0 else hB), [hsc[ex_ % 2][s_]])
    P.barrier()
    P.release(mk)


def phase_P(P, C, K, l, last):
    mk = P.mark()
    ps, psb, bk = C.ps, C.psb, C.bk
    w_g = C.v_w_g
    w_p = C.v_w_p
    g_p = P.sb([128, 1024], F32, "g_p")
    if not last:
        pre_w_in(P, C, l + 1)
    P.dma("sp", lambda e: e.dma_start(out=g_p[:], in_=C.ple_norm[l].partition_broadcast(128)), [], [g_p])
    if last:
        g_fin = P.sb([128, 1024], F32, "g_fin")
        P.dma("sp", lambda e: e.dma_start(out=g_fin[:], in_=C.final_norm.partition_broadcast(128)), [], [g_fin])
    hts = [P.sb([128, 1024], F32, "ht") for _ in range(2)]
    pts = [P.sb([128, 256], BF16, "ptile") for _ in range(2)]
    xns = [P.sb([128, 1024], BF16, "xn") for _ in range(2)]
    xTs = [P.sb([128, 10, 128], BF16, "xT") for _ in range(2)]
    junk = P.sb([128, 1024], BF16, "junk")
    egs = [P.sb([128, 1024], F32, "eg") for _ in range(2)]
    pps = [P.sb([128, 1024], F32, "pp") for _ in range(2)]
    junk2 = P.sb([128, 1024], BF16, "junk2")
    h3s = [P.sb([128, 1024], F32, "h3") for _ in range(2)]
    outs = [P.sb([128, 1024], F32, "outt") for _ in range(2)]
    ss = [P.sb([128, 1], F32, "ss") for _ in range(2)]
    rs = [P.sb([128, 1], F32, "rs") for _ in range(2)]
    ss2 = [P.sb([128, 1], F32, "ss2") for _ in range(2)]
    rs2 = [P.sb([128, 1], F32, "rs2") for _ in range(2)]
    def front(T):
        ht, pt, xn, xT = hts[T % 2], pts[T % 2], xns[T % 2], xTs[T % 2]
        rows = slice(T * 128, (T + 1) * 128)
        P.dma("sp", lambda e, ht=ht, rows=rows: e.dma_start(out=ht[:], in_=C.h_d[rows, :]), [C.B["h"][T]], [ht])
        P.dma("pool", lambda e, pt=pt, rows=rows: e.dma_start(out=pt[:], in_=C.p[l, rows, :]), [], [pt])
        P.op("act", lambda e, ht=ht, T=T: e.activation(out=junk[:], in_=ht[:], func=AF.Square, accum_out=ss[T % 2][:]), [ht], [junk, ss[T % 2]])
        _rstd(P, ss[T % 2], rs[T % 2], 1024)
        P.op("dve", lambda e, ht=ht, xn=xn, T=T: e.scalar_tensor_tensor(out=xn[:], in0=ht[:], scalar=rs[T % 2][:], in1=g_p[:], op0=ALU.mult, op1=ALU.mult),
             [ht, rs[T % 2], g_p], [xn])

    def front_b(T):
        pt, xn, xT = pts[T % 2], xns[T % 2], xTs[T % 2]
        tb = 6 + T % 2
        for c in range(8):
            _tr(P, psb[:, tb * 1024 + c * 128: tb * 1024 + (c + 1) * 128], xn[:, c * 128:(c + 1) * 128], K.ident[:], [xn, K.ident], [bk[tb]])
        P.op("act", lambda e, xT=xT, tb=tb: e.activation(out=xT[:, 0:8, :].rearrange("p c t -> p (c t)"), in_=psb[:, tb * 1024:(tb + 1) * 1024], func=AF.Copy), [bk[tb]], [xT])
        for c in range(2):
            _tr(P, psb[:, 4 * 1024 + c * 128: 4 * 1024 + (c + 1) * 128], pt[:, c * 128:(c + 1) * 128], K.ident[:], [pt, K.ident], [bk[4]])
        P.op("act", lambda e, xT=xT: e.activation(out=xT[:, 8:10, :].rearrange("p c t -> p (c t)"), in_=psb[:, 4 * 1024: 4 * 1024 + 256], func=AF.Copy), [bk[4]], [xT])

    def back_mm(T):
        xT = xTs[T % 2]
        for half in range(2):
            for k in range(8):
                _mm(P, ps[:, half * 512:(half + 1) * 512], xT[:, k, :], w_g[:, k, half * 512:(half + 1) * 512], k == 0, k == 7, [xT, w_g], [bk[half]])
            for k in range(2):
                _mm(P, ps[:, (2 + half) * 512:(3 + half) * 512], xT[:, 8 + k, :], w_p[:, k, half * 512:(half + 1) * 512], k == 0, k == 1, [xT, w_p], [bk[2 + half]])
        for half in range(2):
            hs = slice(half * 512, (half + 1) * 512)
            eg = egs[T % 2]
            pp = pps[T % 2]
            P.op("act", lambda e, half=half, hs=hs, eg=eg: e.activation(out=eg[:, hs], in_=ps[:, half * 512:(half + 1) * 512], func=AF.Exp, scale=-1.0), [bk[half]], [eg])
            P.op("act", lambda e, hs=hs, eg=eg: e.activation(out=eg[:, hs], in_=eg[:, hs], func=AF.Ln, bias=1.0), [eg], [eg])
            P.op("act", lambda e, hs=hs, eg=eg: e.activation(out=eg[:, hs], in_=eg[:, hs], func=AF.Exp, scale=-1.0), [eg], [eg])

    def back(T):
        ht, xT, h3 = hts[T % 2], xTs[T % 2], h3s[T % 2]
        eg = egs[T % 2]
        pp = pps[T % 2]
        rows = slice(T * 128, (T + 1) * 128)
        for half in range(2):
            hs = slice(half * 512, (half + 1) * 512)
            P.op("dve", lambda e, half=half, hs=hs, eg=eg: e.tensor_tensor(out=eg[:, hs], in0=ps[:, (2 + half) * 512:(3 + half) * 512], in1=eg[:, hs], op=ALU.mult), [bk[2 + half], eg], [eg])
            P.op("dve", lambda e, hs=hs, ht=ht, h3=h3, eg=eg: e.tensor_tensor(out=h3[:, hs], in0=eg[:, hs], in1=ht[:, hs], op=ALU.add), [eg, ht], [h3])
        if not last:
            P.dma("sp", lambda e, h3=h3, rows=rows: e.dma_start(out=C.h_d[rows, :], in_=h3[:]), [h3], [C.B["h"][T]])
        else:
            ot = outs[T % 2]
            P.op("act", lambda e, h3=h3, T=T: e.activation(out=junk2[:], in_=h3[:], func=AF.Square, accum_out=ss2[T % 2][:]), [h3], [junk2, ss2[T % 2]])
            _rstd(P, ss2[T % 2], rs2[T % 2], 1024)
            P.op("dve", lambda e, h3=h3, ot=ot, T=T: e.scalar_tensor_tensor(out=ot[:], in0=h3[:], scalar=rs2[T % 2][:], in1=g_fin[:], op0=ALU.mult, op1=ALU.mult),
                 [h3, rs2[T % 2], g_fin], [ot])
            P.dma("sp", lambda e, ot=ot, rows=rows: e.dma_start(out=C.out[rows, :], in_=ot[:]), [ot], [C.B["out"][T]])

    front(0)
    front_b(0)
    for T in range(NT):
        if T + 1 < NT:
            front(T + 1)
        back_mm(T)
        if T + 1 < NT:
            front_b(T + 1)
        back(T)
    P.barrier()
    P.release(mk)


W_NAMES = [("attn_norm", [2, 1024]), ("w_in", [2, 1024, 1952]), ("mla_q_norm", [2, 384]), ("mla_w_q_up", [2, 384, 768]),
           ("mla_kv_norm", [2, 256]), ("mla_w_kv_up", [2, 256, 1024]), ("swa_sink", [2, 4]), ("w_out", [2, 1024, 1024]),
           ("ffn_norm", [2, 1024]), ("w_router", [2, 1024, 16]), ("w_gate_e", [2, 16, 1024, 512]), ("w_up_e", [2, 16, 1024, 512]),
           ("w_down_e", [2, 16, 512, 1024]), ("ple_norm", [2, 1024]), ("w_ple_gate", [2, 1024, 1024]), ("w_ple_proj", [2, 256, 1024]),
           ("final_norm", [1024])]
CONSTS = [("cos_swa", [S, 32]), ("sin_swa", [S, 32]), ("cos_mla", [S, 16]), ("sin_mla", [S, 16]), ("cosT_mla", [32, S]),
          ("sinT_mla", [32, S]), ("nab", [2, 4, 21, 128, 128]), ("namask", [21, 128, 128]), ("swmask", [2, 128, 128])]
SCRATCH = [("h_d", [S, D], F32), ("hnb_d", [S, D], BF16), ("naqT_d", [2, 128, S], BF16), ("nakT_d", [2, 128, S], BF16),
           ("nav_d", [S, 260], BF16), ("cqT_d", [3, 128, S], BF16), ("ckvT_d", [2, 128, S], BF16), ("krT_d", [32, S], BF16),
           ("swqT_d", [2, 128, S], BF16), ("swkT_d", [2, 128, S], BF16), ("swv_d", [S, 130], BF16), ("oT_d", [8, 128, S], BF16)]
PHASES = ["A", "na", "sw", "mla", "O", "R", "E", "P"]


def build(stop=None, dbg=()):
    nc = bass.Bass("TRN2", target_bir_lowering=False)
    C = NS()
    C.x = nc.dram_tensor("x", [S, D], F32, kind="ExternalInput").ap()
    C.p = nc.dram_tensor("p", [2, S, 256], F32, kind="ExternalInput").ap()
    for n, shp in W_NAMES + CONSTS:
        setattr(C, n, nc.dram_tensor(n, list(shp), F32, kind="ExternalInput").ap())
    C.out = nc.dram_tensor("out", [S, D], F32, kind="ExternalOutput").ap()
    for n, shp, dt_ in SCRATCH:
        kind = "ExternalOutput" if n in dbg else "Internal"
        setattr(C, n, nc.dram_tensor(n, list(shp), dt_, kind=kind).ap())
    if "aff_dbg" in dbg:
        C.aff_dbg = nc.dram_tensor("aff_dbg", [128, 512], F32, kind="ExternalOutput").ap()
        C.idx_dbg = nc.dram_tensor("idx_dbg", [128, 64], I32, kind="ExternalOutput").ap()
        C.gsl_dbg = nc.dram_tensor("gsl_dbg", [128, 64], F32, kind="ExternalOutput").ap()
    C.B = {}
    for n in ("x", "h", "hnb", "out"):
        C.B[n] = [Buf(None, n + str(i)) for i in range(NT)]
    for n in ("naqT", "nakT", "nav", "cqT", "ckvT", "krT", "swqT", "swkT", "swv", "oT_na", "oT_mla", "oT_sw"):
        C.B[n] = [Buf(None, n + str(i)) for i in range(NCH)]
    with ExitStack() as es:
        P = Prog(nc, es)
        C.ps = es.enter_context(nc.psum_tensor("ps", [128, 4096], F32))
        C.psb = C.ps.bitcast(BF16)
        C.bk = [Buf(None, "bank%d" % i) for i in range(8)]
        setup_regions(P, C)
        pre_w_in(P, C, 0)
        K = setup_consts(P, C)
        done = False
        for l in range(2):
            for ph in PHASES:
                if ph == "A":
                    phase_A(P, C, K, l)
                elif ph in ("na", "sw"):
                    phase_local(P, C, K, l, ph)
                elif ph == "mla":
                    phase_mla(P, C, K, l)
                elif ph == "O":
                    phase_O(P, C, K, l)
                elif ph == "R":
                    phase_R(P, C, K, l)
                    if "aff_dbg" in dbg and l == 0:
                        P.dma("sp", lambda e: e.dma_start(out=C.aff_dbg, in_=K.aff[:].rearrange("p i e -> p (i e)")), [K.aff], [])
                        P.dma("sp", lambda e: e.dma_start(out=C.idx_dbg, in_=K.idx[:].rearrange("p e s -> p (e s)")), [K.idx], [])
                        P.dma("sp", lambda e: e.dma_start(out=C.gsl_dbg, in_=K.gsl[:].rearrange("p e s -> p (e s)")), [K.gsl], [])
                elif ph == "E":
                    phase_E(P, C, K, l)
                elif ph == "P":
                    phase_P(P, C, K, l, l == 1)
                if stop == (l, ph):
                    done = True
                    break
            if done:
                break
        P.finish()
        print("n_inst", P.n_inst, "sbuf_hi", P.sb_hi, flush=True)
    return nc


def _rope_tab(dim):
    inv = (1.0 / (10000.0 ** (np.arange(0, dim, 2, dtype=np.float32) / np.float32(dim)))).astype(np.float32)
    ang = np.arange(S, dtype=np.float32)[:, None] * inv[None, :]
    return np.cos(ang).astype(np.float32), np.sin(ang).astype(np.float32)


def _na_tables():
    W = 64
    kl = np.arange(128)
    krl, kc = kl // W, kl % W
    qrl, qc = kl // W, kl % W
    dri = np.zeros((21, 128, 128), np.int64)
    dci = np.zeros((21, 128, 128), np.int64)
    msk = np.zeros((21, 128, 128), np.float32)
    for kd, (j, d) in enumerate(NA_KINDS):
        kt = j + d
        qr = 2 * j + qrl[None, :]
        kr = 2 * kt + krl[:, None]
        r0 = np.clip(qr - 4, 0, 56)
        row_ok = (kr >= r0) & (kr < r0 + 8)
        c0 = np.clip(qc[None, :] - 8, 0, 48)
        col_ok = (kc[:, None] >= c0) & (kc[:, None] < c0 + 16)
        dri[kd] = np.clip(kr - qr + 7, 0, 14)
        dci[kd] = np.clip(kc[:, None] - qc[None, :], -15, 15) + 15
        msk[kd] = np.where(row_ok & col_ok, 0.0, NEG)
    return dri, dci, msk


def _sw_masks():
    kl = np.arange(128)[:, None]
    ql = np.arange(128)[None, :]
    m = np.zeros((2, 128, 128), np.float32)
    m[0] = np.where(kl >= ql, 0.0, NEG)
    m[1] = np.where(kl <= ql, 0.0, NEG)
    return m


_NC_CACHE = {}


def host_inputs(inputs, cores):
    cs, sn = _rope_tab(64)
    cm, sm = _rope_tab(32)
    dri, dci, msk = _na_tables()
    rpb = np.asarray(inputs["na_rpb"], np.float32)
    nab = np.ascontiguousarray(rpb[:, :, dri, dci])
    shared = {n: np.ascontiguousarray(np.asarray(inputs[n], np.float32)) for n, _ in W_NAMES}
    shared.update(cos_swa=cs, sin_swa=sn, cos_mla=cm, sin_mla=sm,
                  cosT_mla=np.ascontiguousarray(np.concatenate([cm, cm], 1).T),
                  sinT_mla=np.ascontiguousarray(np.concatenate([sm, sm], 1).T),
                  nab=nab, namask=msk, swmask=_sw_masks())
    x = np.asarray(inputs["x"], np.float32)
    p = np.asarray(inputs["p"], np.float32)
    maps = []
    for b in cores:
        m = dict(shared)
        m["x"] = np.ascontiguousarray(x[b])
        m["p"] = np.ascontiguousarray(p[:, b])
        maps.append(m)
    return maps


def kernel(**inputs):
    if "nc" not in _NC_CACHE:
        _NC_CACHE["nc"] = build()
    nc = _NC_CACHE["nc"]
    maps = host_inputs(inputs, list(range(8)))
    res = run_bass_kernel_spmd(nc, maps, core_ids=list(range(8)))
    return np.stack([np.asarray(r["out"], np.float32) for r in res.results], axis=0)
```

```python
import numpy as np
import concourse.bass as bass
import concourse.mybir as mybir
from contextlib import ExitStack

F32 = mybir.dt.float32
BF16 = mybir.dt.bfloat16
I32 = mybir.dt.int32
ALU = mybir.AluOpType
AF = mybir.ActivationFunctionType
AX = mybir.AxisListType

ENGS = ("pe", "act", "dve", "pool", "sp")
N_DMA_SEMS = 32
SB_BASE = 16512
DBG_CUT = None
SB_END = 229312
PF1_BASE = SB_END - 32768
PF2_BASE = SB_END - 65536


class Buf:
    __slots__ = ("t", "w", "r", "name")

    def __init__(self, t, name=""):
        self.t = t
        self.w = None
        self.r = {}
        self.name = name

    def __getitem__(self, idx):
        return self.t[idx]


class PFView(Buf):
    __slots__ = ("base",)

    def __init__(self, base, ap, name=""):
        self.base = base
        self.t = ap
        self.name = name

    w = property(lambda s: s.base.w, lambda s, v: setattr(s.base, "w", v))
    r = property(lambda s: s.base.r, lambda s, v: setattr(s.base, "r", v))


class Prog:
    def __init__(self, nc, es):
        self.nc = nc
        self.es = es
        self.q = {e: [] for e in ENGS}
        self.cnt = {e: 0 for e in ENGS}
        self.sems = {}
        for e in ENGS:
            self.sems[e] = es.enter_context(nc.semaphore("s_" + e))
        self.dsem_cnt = []
        for i in range(N_DMA_SEMS):
            k = "d%d" % i
            self.sems[k] = es.enter_context(nc.semaphore("s_" + k))
            self.dsem_cnt.append(0)
        self.dnext = 0
        self.dnext2 = 0
        self.known = {e: {} for e in ENGS}
        self.sb_off = SB_BASE
        self.sb_hi = SB_BASE
        self.uid = 0
        self.n_inst = 0

    def sb(self, shape, dtype, name=None):
        self.uid += 1
        nm = "%s_%d" % (name or "sb", self.uid)
        esz = {F32: 4, BF16: 2, I32: 4, mybir.dt.int16: 2}[dtype]
        per = int(np.prod(shape[1:])) * esz
        off = (self.sb_off + 31) // 32 * 32
        t = self.nc.alloc_sbuf_tensor_at(nm, list(shape), dtype, offset=off)
        self.sb_off = off + per
        self.sb_hi = max(self.sb_hi, self.sb_off)
        assert self.sb_off <= PF1_BASE, ("SBUF overflow", self.sb_off)
        return Buf(t, nm)

    def mark(self):
        return self.sb_off

    def release(self, mark):
        self.sb_off = mark

    def _deps(self, eng, reads, writes):
        deps = {}

        def add(k, v):
            if deps.get(k, 0) < v:
                deps[k] = v
        for b in reads:
            if b.w is not None:
                add(*b.w)
        for b in writes:
            if b.w is not None:
                add(*b.w)
            for k, v in b.r.items():
                add(k, v)
        if eng == "pe":
            deps.pop("pe", None)
        out = []
        kn = self.known[eng]
        for k, v in deps.items():
            if kn.get(k, 0) < v:
                kn[k] = v
                out.append((k, v))
        return out

    def _emit_waits(self, eng, deps):
        for k, v in deps:
            s = self.sems[k]
            self.q[eng].append(lambda e, s=s, v=v: e.wait_ge(s, v))

    def op(self, eng, fn, reads=(), writes=()):
        if DBG_CUT is not None and self.n_inst >= DBG_CUT:
            return
        deps = self._deps(eng, reads, writes)
        self._emit_waits(eng, deps)
        self.cnt[eng] += 1
        val = self.cnt[eng]
        s = self.sems[eng]
        self.q[eng].append(lambda e, fn=fn, s=s: fn(e).then_inc(s, 1))
        for b in writes:
            b.w = (eng, val)
            b.r = {}
        for b in reads:
            if b.r.get(eng, 0) < val:
                b.r[eng] = val
        self.n_inst += 1

    def dma(self, eng, fn, reads=(), writes=()):
        if DBG_CUT is not None and self.n_inst >= DBG_CUT:
            return
        half = N_DMA_SEMS // 2
        if eng == "sp":
            i = self.dnext
            self.dnext = (self.dnext + 1) % half
        else:
            i = half + self.dnext2
            self.dnext2 = (self.dnext2 + 1) % half
        k = "d%d" % i
        deps = self._deps(eng, reads, writes)
        prev = self.dsem_cnt[i]
        if prev and self.known[eng].get(k, 0) < prev:
            self.known[eng][k] = prev
            deps.append((k, prev))
        self._emit_waits(eng, deps)
        self.dsem_cnt[i] += 16
        val = self.dsem_cnt[i]
        s = self.sems[k]
        self.q[eng].append(lambda e, fn=fn, s=s: fn(e).then_inc(s, 16))
        for b in writes:
            b.w = (k, val)
            b.r = {}
        for b in reads:
            if b.r.get(k, 0) < val:
                b.r[k] = val
        self.n_inst += 1

    def barrier(self):
        for e in ENGS:
            deps = []
            for e2 in ENGS:
                if e2 != e and self.cnt[e2] > self.known[e].get(e2, 0):
                    self.known[e][e2] = self.cnt[e2]
                    deps.append((e2, self.cnt[e2]))
            for i in range(N_DMA_SEMS):
                k = "d%d" % i
                v = self.dsem_cnt[i]
                if v > self.known[e].get(k, 0):
                    self.known[e][k] = v
                    deps.append((k, v))
            self._emit_waits(e, deps)

    def finish(self):
        nc = self.nc
        self.barrier()
        with nc.Block() as block:
            @block.tensor
            def _(e):
                for f in self.q["pe"]:
                    f(e)

            @block.scalar
            def _(e):
                for f in self.q["act"]:
                    f(e)

            @block.vector
            def _(e):
                for f in self.q["dve"]:
                    f(e)

            @block.gpsimd
            def _(e):
                for f in self.q["pool"]:
                    f(e)

            @block.sync
            def _(e):
                for f in self.q["sp"]:
                    f(e)


from concourse.bass_utils import run_bass_kernel_spmd

S = 4096
D = 1024
NT = 32
NCH = 8
EPS = 1e-6
NEG = -30000.0
DBG_NT = None
GR = [(0, 512), (512, 768), (768, 1152), (1152, 1440), (1440, 1952)]


class NS:
    pass


def _mm(P, out, lhsT, rhs, start, stop, reads, writes):
    P.op("pe", lambda e: e.matmul(out, lhsT=lhsT, rhs=rhs, start=start, stop=stop), reads, writes)


def _tr(P, out, in_, ident, reads, writes):
    P.op("pe", lambda e: e.transpose(out=out, in_=in_, identity=ident), reads, writes)


def _rstd(P, ss, rstd, n):
    P.op("act", lambda e: e.activation(out=rstd[:], in_=ss[:], func=AF.Ln, scale=1.0 / n, bias=EPS), [ss], [rstd])
    P.op("act", lambda e: e.activation(out=rstd[:], in_=rstd[:], func=AF.Exp, scale=-0.5), [rstd], [rstd])


def setup_regions(P, C):
    nc = P.nc
    t1 = nc.alloc_sbuf_tensor_at("pf1", [128, 16384], BF16, offset=PF1_BASE)
    t2 = nc.alloc_sbuf_tensor_at("pf2", [128, 16384], BF16, offset=PF2_BASE)
    C.pf1 = Buf(t1, "pf1")
    C.pf2 = Buf(t2, "pf2")
    C.v_w_in = PFView(C.pf2, t2[:, 0:15616].rearrange("p (c n) -> p c n", c=8), "v_w_in")
    C.v_w_out = PFView(C.pf1, t1[:, 0:8192].rearrange("p (c n) -> p c n", c=8), "v_w_out")
    C.v_w_r = PFView(C.pf1, t1[:, 8192:8320].rearrange("p (c n) -> p c n", c=8), "v_w_r")
    C.v_w_g = PFView(C.pf1, t1[:, 0:8192].rearrange("p (c n) -> p c n", c=8), "v_w_g")
    C.v_w_p = PFView(C.pf1, t1[:, 8192:10240].rearrange("p (c n) -> p c n", c=2), "v_w_p")
    C.v_wg0 = PFView(C.pf1, t1[:, 0:4096].rearrange("p (c n) -> p c n", c=8), "v_wg0")
    C.v_wu0 = PFView(C.pf1, t1[:, 4096:8192].rearrange("p (c n) -> p c n", c=8), "v_wu0")
    C.v_wd0 = PFView(C.pf1, t1[:, 8192:12288].rearrange("p (c n) -> p c n", c=4), "v_wd0")


def pre_w_in(P, C, l):
    v = C.v_w_in
    P.dma("pool", lambda e: e.dma_start(out=v[:], in_=C.w_in[l].rearrange("(c p) n -> p c n", p=128)), [], [v])


def pre_w_out(P, C, l):
    v, r = C.v_w_out, C.v_w_r
    P.dma("pool", lambda e: e.dma_start(out=v[:], in_=C.w_out[l].rearrange("(c p) n -> p c n", p=128)), [], [v])
    P.dma("pool", lambda e: e.dma_start(out=r[:], in_=C.w_router[l].rearrange("(c p) n -> p c n", p=128)), [], [r])


def pre_exp0(P, C, l):
    P.dma("pool", lambda e: e.dma_start(out=C.v_wg0[:], in_=C.w_gate_e[l, 0].rearrange("(c p) n -> p c n", p=128)), [], [C.v_wg0])
    P.dma("pool", lambda e: e.dma_start(out=C.v_wu0[:], in_=C.w_up_e[l, 0].rearrange("(c p) n -> p c n", p=128)), [], [C.v_wu0])
    P.dma("pool", lambda e: e.dma_start(out=C.v_wd0[:], in_=C.w_down_e[l, 0].rearrange("(c p) n -> p c n", p=128)), [], [C.v_wd0])


def pre_ple(P, C, l):
    P.dma("pool", lambda e: e.dma_start(out=C.v_w_g[:], in_=C.w_ple_gate[l].rearrange("(c p) n -> p c n", p=128)), [], [C.v_w_g])
    P.dma("pool", lambda e: e.dma_start(out=C.v_w_p[:], in_=C.w_ple_proj[l].rearrange("(c p) n -> p c n", p=128)), [], [C.v_w_p])


def setup_consts(P, C):
    K = NS()
    K.identf = P.sb([128, 128], F32, "identf")
    K.ident = P.sb([128, 128], BF16, "ident")
    K.ones = P.sb([128, 128], BF16, "ones")
    K.onesf = P.sb([128, 64], F32, "onesf")
    K.L = P.sb([128, 128], BF16, "Ltri")
    K.Lf = P.sb([128, 128], F32, "Lf")
    K.iota_i = P.sb([128, 512], I32, "iota_i")
    K.iota = P.sb([128, 512], F32, "iota")
    K.iota16 = P.sb([128, 512], mybir.dt.int16, "iota16")
    K.pcol_i = P.sb([128, 1], I32, "pcol_i")
    K.pcol = P.sb([128, 1], F32, "pcol")
    K.aff = P.sb([128, 32, 16], F32, "aff")
    K.idx = P.sb([128, 16, 4], I32, "idx")
    K.gsl = P.sb([128, 16, 4], F32, "gsl")
    P.op("pool", lambda e: e.memset(K.identf[:], 0.0), [], [K.identf])
    P.op("pool", lambda e: e.affine_select(out=K.identf[:], in_=K.identf[:], pattern=[[-1, 128]],
                                           compare_op=ALU.not_equal, fill=1.0, base=0, channel_multiplier=1),
         [K.identf], [K.identf])
    P.op("dve", lambda e: e.tensor_copy(out=K.ident[:], in_=K.identf[:]), [K.identf], [K.ident])
    P.op("dve", lambda e: e.memset(K.ones[:], 1.0), [], [K.ones])
    P.op("dve", lambda e: e.memset(K.onesf[:], 1.0), [], [K.onesf])
    P.op("pool", lambda e: e.memset(K.Lf[:], 0.0), [], [K.Lf])
    P.op("pool", lambda e: e.affine_select(out=K.Lf[:], in_=K.Lf[:], pattern=[[-1, 128]],
                                           compare_op=ALU.is_ge, fill=1.0, base=0, channel_multiplier=1),
         [K.Lf], [K.Lf])
    P.op("dve", lambda e: e.tensor_copy(out=K.L[:], in_=K.Lf[:]), [K.Lf], [K.L])
    P.op("pool", lambda e: e.iota(K.iota_i[:], pattern=[[1, 512]], base=0, channel_multiplier=0), [], [K.iota_i])
    P.op("dve", lambda e: e.tensor_copy(out=K.iota[:], in_=K.iota_i[:]), [K.iota_i], [K.iota])
    P.op("dve", lambda e: e.tensor_copy(out=K.iota16[:], in_=K.iota_i[:]), [K.iota_i], [K.iota16])
    P.op("pool", lambda e: e.iota(K.pcol_i[:], pattern=[[0, 1]], base=0, channel_multiplier=1), [], [K.pcol_i])
    P.op("dve", lambda e: e.tensor_copy(out=K.pcol[:], in_=K.pcol_i[:]), [K.pcol_i], [K.pcol])
    return K


def phase_A(P, C, K, l):
    mk = P.mark()
    ps, psb, bk = C.ps, C.psb, C.bk
    w_in = C.v_w_in
    g_at = P.sb([128, 1024], F32, "g_at")
    g_q = P.sb([128, 384], F32, "g_q")
    g_kv = P.sb([128, 256], F32, "g_kv")
    P.dma("sp", lambda e: e.dma_start(out=g_at[:], in_=C.attn_norm[l].partition_broadcast(128)), [], [g_at])
    P.dma("sp", lambda e: e.dma_start(out=g_q[:], in_=C.mla_q_norm[l].partition_broadcast(128)), [], [g_q])
    P.dma("sp", lambda e: e.dma_start(out=g_kv[:], in_=C.mla_kv_norm[l].partition_broadcast(128)), [], [g_kv])
    cos_sw = P.sb([128, 32, 32], F32, "cos_sw")
    sin_sw = P.sb([128, 32, 32], F32, "sin_sw")
    cos_ml = P.sb([128, 32, 16], F32, "cos_ml")
    sin_ml = P.sb([128, 32, 16], F32, "sin_ml")
    for t, src in ((cos_sw, C.cos_swa), (sin_sw, C.sin_swa), (cos_ml, C.cos_mla), (sin_ml, C.sin_mla)):
        P.dma("sp", lambda e, t=t, src=src: e.dma_start(out=t[:], in_=src.rearrange("(t p) d -> p t d", p=128)), [], [t])
    hts = [P.sb([128, 1024], F32, "ht") for _ in range(3)]
    junk = P.sb([128, 1024], BF16, "junk")
    xns = [P.sb([128, 1024], BF16, "xn") for _ in range(3)]
    xTs = [P.sb([128, 8, 128], BF16, "xT") for _ in range(2)]
    ss = [P.sb([128, 1], F32, "ss") for _ in range(3)]
    rs = [P.sb([128, 1], F32, "rstd") for _ in range(3)]
    ssq = P.sb([128, 1], F32, "ssq")
    rq = P.sb([128, 1], F32, "rq")
    sskv = P.sb([128, 1], F32, "sskv")
    rkv = P.sb([128, 1], F32, "rkv")
    naqk = P.sb([128, 512], BF16, "naqk")
    cqn = P.sb([128, 384], BF16, "cqn")
    ckvn = P.sb([128, 256], BF16, "ckvn")
    kr96 = P.sb([128, 96], BF16, "kr96")
    swqk = P.sb([128, 8, 64], BF16, "swqk")
    ra = P.sb([128, 384], F32, "ra")
    krt = P.sb([128, 2, 64], BF16, "krt")
    rb = P.sb([128, 384], F32, "rb")
    st = []
    for _ in range(2):
        s = NS()
        s.naqk = P.sb([128, 4, 512], BF16, "st_naqk")
        s.nav = P.sb([128, 4, 4, 65], BF16, "st_nav")
        s.cq = P.sb([128, 3, 512], BF16, "st_cq")
        s.ckv = P.sb([128, 2, 512], BF16, "st_ckv")
        s.kr = P.sb([128, 512], BF16, "st_kr")
        s.swqk = P.sb([128, 4, 512], BF16, "st_swqk")
        s.swv = P.sb([128, 4, 2, 65], BF16, "st_swv")
        P.op("pool", lambda e, s=s: e.memset(s.nav[:], 1.0), [], [s.nav])
        P.op("pool", lambda e, s=s: e.memset(s.swv[:], 1.0), [], [s.swv])
        st.append(s)
    P.op("pool", lambda e: e.memset(kr96[:], 0.0), [], [kr96])
    src = C.x if l == 0 else C.h_d
    srcB = C.B["x"] if l == 0 else C.B["h"]
    b5, b6, b7 = bk[5], bk[6], bk[7]

    def bf(bank, c0, c1):
        return psb[:, bank * 1024 + c0: bank * 1024 + c1]

    gss = [P.sb([128, 1056], F32, "gs") for _ in range(2)]
    naqks = [naqk, P.sb([128, 512], BF16, "naqk2")]
    NTT = DBG_NT or NT

    def fa(T):
        ht, xn = hts[T % 3], xns[T % 3]
        P.dma("sp", lambda e, ht=ht, T=T: e.dma_start(out=ht[:], in_=src[T * 128:(T + 1) * 128, :]), [srcB[T]], [ht])
        P.op("act", lambda e, ht=ht, T=T: e.activation(out=junk[:], in_=ht[:], func=AF.Square, accum_out=ss[T % 3][:]),
             [ht], [junk, ss[T % 3]])
        _rstd(P, ss[T % 3], rs[T % 3], 1024)
        P.op("dve", lambda e, ht=ht, xn=xn, T=T: e.scalar_tensor_tensor(out=xn[:], in0=ht[:], scalar=rs[T % 3][:], in1=g_at[:],
                                                                      op0=ALU.mult, op1=ALU.mult), [ht, rs[T % 3], g_at], [xn])

    def fb(T):
        xn, xT = xns[T % 3], xTs[T % 2]
        for c in range(8):
            _tr(P, bf(5, c * 128, (c + 1) * 128), xn[:, c * 128:(c + 1) * 128], K.ident[:], [xn, K.ident], [b5])
        P.op("act", lambda e, xT=xT: e.activation(out=xT[:].rearrange("p c t -> p (c t)"), in_=bf(5, 0, 1024), func=AF.Copy), [b5], [xT])

    def mm(T):
        xT = xTs[T % 2]
        for gi, (c0, c1) in enumerate(GR):
            for k in range(8):
                _mm(P, ps[:, gi * 512: gi * 512 + (c1 - c0)], xT[:, k, :], w_in[:, k, c0:c1], k == 0, k == 7, [xT, w_in], [bk[gi]])

    def gc(T):
        ch, tt = T // 4, T % 4
        s = st[ch % 2]
        gs = gss[T % 2]
        nq = naqks[T % 2]
        P.op("act", lambda e, nq=nq: e.activation(out=nq[:], in_=ps[:, 0:512], func=AF.Copy), [bk[0]], [nq])
        P.op("dve", lambda e, gs=gs: e.tensor_copy(out=gs[:, 0:384], in_=ps[:, 1024:1408]), [bk[2]], [gs])
        P.op("dve", lambda e, gs=gs: e.tensor_copy(out=gs[:, 672:1056], in_=ps[:, 2048:2432]), [bk[4]], [gs])
        P.op("act", lambda e, gs=gs: e.activation(out=gs[:, 384:672], in_=ps[:, 1536:1824], func=AF.Copy), [bk[3]], [gs])
        P.op("dve", lambda e, s=s, tt=tt: e.tensor_copy(out=s.nav[:, tt, :, 0:64], in_=ps[:, 512:768].rearrange("p (h d) -> p h d", h=4)), [bk[1]], [s.nav])
        P.op("act", lambda e, s=s, tt=tt: e.activation(out=s.swv[:, tt, :, 0:64], in_=ps[:, 2432:2560].rearrange("p (h d) -> p h d", h=2),
                                                       func=AF.Copy), [bk[4]], [s.swv])

    def post_a(T):
        gs = gss[T % 2]
        P.op("act", lambda e, gs=gs: e.activation(out=junk2[:, 0:384], in_=gs[:, 0:384], func=AF.Square, accum_out=ssq[:]), [gs], [junk2, ssq])
        _rstd(P, ssq, rq, 384)
        P.op("dve", lambda e, gs=gs: e.scalar_tensor_tensor(out=cqn[:], in0=gs[:, 0:384], scalar=rq[:], in1=g_q[:],
                                                     op0=ALU.mult, op1=ALU.mult), [gs, rq, g_q], [cqn])
        P.op("act", lambda e, gs=gs: e.activation(out=junk2[:, 384:640], in_=gs[:, 384:640], func=AF.Square, accum_out=sskv[:]), [gs], [junk2, sskv])
        _rstd(P, sskv, rkv, 256)
        P.op("dve", lambda e, gs=gs: e.scalar_tensor_tensor(out=ckvn[:], in0=gs[:, 384:640], scalar=rkv[:], in1=g_kv[:],
                                                     op0=ALU.mult, op1=ALU.mult), [gs, rkv, g_kv], [ckvn])
        xk = gs[:, 640:672].rearrange("p (a d) -> p a d", a=2)
        P.op("dve", lambda e, T=T, xk=xk: e.tensor_tensor(out=ra[:, 0:32].rearrange("p (a d) -> p a d", a=2), in0=xk,
                                                   in1=cos_ml[:, T, :].unsqueeze(1).broadcast_to([128, 2, 16]), op=ALU.mult),
             [gs, cos_ml], [ra])
        P.op("dve", lambda e, T=T, xk=xk: e.tensor_tensor(out=rb[:, 0:32].rearrange("p (a d) -> p a d", a=2), in0=xk,
                                                   in1=sin_ml[:, T, :].unsqueeze(1).broadcast_to([128, 2, 16]), op=ALU.mult),
             [gs, sin_ml], [rb])
        P.op("dve", lambda e: e.tensor_tensor(out=kr96[:, 64:80], in0=ra[:, 0:16], in1=rb[:, 16:32], op=ALU.subtract), [ra, rb], [kr96])
        P.op("dve", lambda e: e.tensor_tensor(out=kr96[:, 80:96], in0=ra[:, 16:32], in1=rb[:, 0:16], op=ALU.add), [ra, rb], [kr96])
        xs_ = gs[:, 672:1056].rearrange("p (h a d) -> p h a d", h=6, a=2)
        P.op("dve", lambda e, T=T, xs_=xs_: e.tensor_tensor(out=ra[:].rearrange("p (h a d) -> p h a d", h=6, a=2), in0=xs_,
                                                   in1=cos_sw[:, T, :].unsqueeze(1).unsqueeze(1).broadcast_to([128, 6, 2, 32]), op=ALU.mult),
             [gs, cos_sw], [ra])
        P.op("dve", lambda e, T=T, xs_=xs_: e.tensor_tensor(out=rb[:].rearrange("p (h a d) -> p h a d", h=6, a=2), in0=xs_,
                                                   in1=sin_sw[:, T, :].unsqueeze(1).unsqueeze(1).broadcast_to([128, 6, 2, 32]), op=ALU.mult),
             [gs, sin_sw], [rb])
        ra4 = ra[:].rearrange("p (h a d) -> p h a d", h=6, a=2)
        rb4 = rb[:].rearrange("p (h a d) -> p h a d", h=6, a=2)
        P.op("dve", lambda e: e.tensor_tensor(out=swqk[:, 0:4, 0:32], in0=ra4[:, 0:4, 0, :], in1=rb4[:, 0:4, 1, :], op=ALU.subtract), [ra, rb], [swqk])
        P.op("dve", lambda e: e.tensor_tensor(out=swqk[:, 0:4, 32:64], in0=ra4[:, 0:4, 1, :], in1=rb4[:, 0:4, 0, :], op=ALU.add), [ra, rb], [swqk])
        P.op("dve", lambda e: e.tensor_tensor(out=krt[:, :, 0:32], in0=ra4[:, 4:6, 0, :], in1=rb4[:, 4:6, 1, :], op=ALU.subtract), [ra, rb], [krt])
        P.op("dve", lambda e: e.tensor_tensor(out=krt[:, :, 32:64], in0=ra4[:, 4:6, 1, :], in1=rb4[:, 4:6, 0, :], op=ALU.add), [ra, rb], [krt])
        P.op("pool", lambda e: e.tensor_copy(out=swqk[:, 4:8, :].rearrange("p (g u) d -> p g u d", u=2),
                                             in_=krt[:].unsqueeze(2).broadcast_to([128, 2, 2, 64])), [krt], [swqk])

    def post_b(T):
        ch, tt = T // 4, T % 4
        s = st[ch % 2]
        nq = naqks[T % 2]
        for cb in range(4):
            _tr(P, bf(6, cb * 128, (cb + 1) * 128), nq[:, cb * 128:(cb + 1) * 128], K.ident[:], [nq, K.ident], [b6])
        for cb in range(3):
            _tr(P, bf(7, cb * 128, (cb + 1) * 128), cqn[:, cb * 128:(cb + 1) * 128], K.ident[:], [cqn, K.ident], [b7])
        for cb in range(2):
            _tr(P, bf(7, (3 + cb) * 128, (4 + cb) * 128), ckvn[:, cb * 128:(cb + 1) * 128], K.ident[:], [ckvn, K.ident], [b7])
        _tr(P, psb[0:96, 7 * 1024 + 5 * 128: 7 * 1024 + 6 * 128], kr96[:, 0:96], K.ident[:], [kr96, K.ident], [b7])
        swf = swqk[:].rearrange("p h d -> p (h d)")
        for cb in range(4):
            _tr(P, bf(6, (4 + cb) * 128, (5 + cb) * 128), swf[:, cb * 128:(cb + 1) * 128], K.ident[:], [swqk, K.ident], [b6])
        tsl = slice(tt * 128, (tt + 1) * 128)
        P.op("act", lambda e, s=s, tsl=tsl: e.activation(out=s.naqk[:, 0:2, tsl], in_=bf(6, 0, 256).rearrange("p (c t) -> p c t", c=2),
                                                         func=AF.Copy, scale=0.125), [b6], [s.naqk])
        P.op("act", lambda e, s=s, tsl=tsl: e.activation(out=s.naqk[:, 2:4, tsl], in_=bf(6, 256, 512).rearrange("p (c t) -> p c t", c=2), func=AF.Copy), [b6], [s.naqk])
        P.op("act", lambda e, s=s, tsl=tsl: e.activation(out=s.swqk[:, 0:2, tsl], in_=bf(6, 512, 768).rearrange("p (c t) -> p c t", c=2),
                                                         func=AF.Copy, scale=0.125), [b6], [s.swqk])
        P.op("act", lambda e, s=s, tsl=tsl: e.activation(out=s.swqk[:, 2:4, tsl], in_=bf(6, 768, 1024).rearrange("p (c t) -> p c t", c=2), func=AF.Copy), [b6], [s.swqk])
        P.op("act", lambda e, s=s, tsl=tsl: e.activation(out=s.cq[:, :, tsl], in_=bf(7, 0, 384).rearrange("p (c t) -> p c t", c=3), func=AF.Copy), [b7], [s.cq])
        P.op("act", lambda e, s=s, tsl=tsl: e.activation(out=s.ckv[:, :, tsl], in_=bf(7, 384, 640).rearrange("p (c t) -> p c t", c=2), func=AF.Copy), [b7], [s.ckv])
        P.op("act", lambda e, s=s, tsl=tsl: e.activation(out=s.kr[64:96, tsl], in_=psb[64:96, 7 * 1024 + 640: 7 * 1024 + 768], func=AF.Copy), [b7], [s.kr])
        if tt == 3:
            cs = slice(ch * 512, (ch + 1) * 512)
            rs_ = slice(ch * 512, (ch + 1) * 512)
            P.dma("sp", lambda e, s=s, cs=cs: e.dma_start(out=C.naqT_d[:, :, cs].rearrange("c p t -> p c t"), in_=s.naqk[:, 0:2, :]), [s.naqk], [C.B["naqT"][ch]])
            P.dma("sp", lambda e, s=s, cs=cs: e.dma_start(out=C.nakT_d[:, :, cs].rearrange("c p t -> p c t"), in_=s.naqk[:, 2:4, :]), [s.naqk], [C.B["nakT"][ch]])
            P.dma("sp", lambda e, s=s, rs_=rs_: e.dma_start(out=C.nav_d[rs_, :].rearrange("(t p) c -> p t c", p=128), in_=s.nav[:].rearrange("p t h d -> p t (h d)")), [s.nav], [C.B["nav"][ch]])
            P.dma("sp", lambda e, s=s, cs=cs: e.dma_start(out=C.cqT_d[:, :, cs].rearrange("c p t -> p c t"), in_=s.cq[:]), [s.cq], [C.B["cqT"][ch]])
            P.dma("sp", lambda e, s=s, cs=cs: e.dma_start(out=C.ckvT_d[:, :, cs].rearrange("c p t -> p c t"), in_=s.ckv[:]), [s.ckv], [C.B["ckvT"][ch]])
            P.dma("sp", lambda e, s=s, cs=cs: e.dma_start(out=C.krT_d[:, cs], in_=s.kr[64:96, :]), [s.kr], [C.B["krT"][ch]])
            P.dma("sp", lambda e, s=s, cs=cs: e.dma_start(out=C.swqT_d[:, :, cs].rearrange("c p t -> p c t"), in_=s.swqk[:, 0:2, :]), [s.swqk], [C.B["swqT"][ch]])
            P.dma("sp", lambda e, s=s, cs=cs: e.dma_start(out=C.swkT_d[:, :, cs].rearrange("c p t -> p c t"), in_=s.swqk[:, 2:4, :]), [s.swqk], [C.B["swkT"][ch]])
            P.dma("sp", lambda e, s=s, rs_=rs_: e.dma_start(out=C.swv_d[rs_, :].rearrange("(t p) c -> p t c", p=128), in_=s.swv[:].rearrange("p t h d -> p t (h d)")), [s.swv], [C.B["swv"][ch]])

    junk2 = P.sb([128, 640], BF16, "junk2")
    fa(0)
    if NTT > 1:
        fa(1)
    fb(0)
    for T in range(NTT):
        if T + 2 < NTT:
            fa(T + 2)
        if T + 1 < NTT:
            fb(T + 1)
        mm(T)
        if T >= 1:
            post_a(T - 1)
        gc(T)
        if T >= 1:
            post_b(T - 1)
    post_a(NTT - 1)
    post_b(NTT - 1)
    P.barrier()
    P.release(mk)


NA_KINDS = [(10, d) for d in (-2, -1, 0, 1, 2)] + [(0, d) for d in (0, 1, 2, 3)] + [(1, d) for d in (-1, 0, 1, 2)] + \
           [(30, d) for d in (-2, -1, 0, 1)] + [(31, d) for d in (-3, -2, -1, 0)]


def na_blocks(j):
    if j == 0:
        return [(d, 5 + d) for d in range(4)]
    if j == 1:
        return [(1 + d, 9 + (d + 1)) for d in (-1, 0, 1, 2)]
    if j == 30:
        return [(30 + d, 13 + (d + 2)) for d in (-2, -1, 0, 1)]
    if j == 31:
        return [(31 + d, 17 + (d + 3)) for d in (-3, -2, -1, 0)]
    return [(j + d, d + 2) for d in (-2, -1, 0, 1, 2)]


def sw_blocks(j):
    out = []
    if j > 0:
        out.append((j - 1, 0))
    out.append((j, None))
    if j < NT - 1:
        out.append((j + 1, 1))
    return out


def phase_local(P, C, K, l, kind):
    mk = P.mark()
    if kind == "na":
        pre_w_out(P, C, l)
    ps, psb, bk = C.ps, C.psb, C.bk
    na = kind == "na"
    H = 4
    HV = 4 if na else 2
    qT = P.sb([128, 2, S], BF16, "qT")
    kT = P.sb([128, 2, S], BF16, "kT")
    v = P.sb([128, NT, HV * 65], BF16, "v")
    qd, kd, vd = (C.naqT_d, C.nakT_d, C.nav_d) if na else (C.swqT_d, C.swkT_d, C.swv_d)
    qB, kB, vB = (C.B["naqT"], C.B["nakT"], C.B["nav"]) if na else (C.B["swqT"], C.B["swkT"], C.B["swv"])
    for c in range(2):
        P.dma("sp", lambda e, c=c: e.dma_start(out=qT[:, c, :], in_=qd[c]), qB, [qT])
        P.dma("sp", lambda e, c=c: e.dma_start(out=kT[:, c, :], in_=kd[c]), kB, [kT])
    P.dma("sp", lambda e: e.dma_start(out=v[:], in_=vd.rearrange("(t p) c -> p t c", p=128)), vB, [v])
    if na:
        nbk = P.sb([128, 4, 21, 128], BF16, "nbk")
        msk = P.sb([128, 21, 128], F32, "msk")
        stg = P.sb([128, 21, 128], F32, "stg")
        P.dma("sp", lambda e: e.dma_start(out=msk[:], in_=C.namask.rearrange("k p q -> p k q")), [], [msk])
        for h in range(4):
            P.dma("sp", lambda e, h=h: e.dma_start(out=stg[:], in_=C.nab[l, h].rearrange("k p q -> p k q")), [], [stg])
            P.op("dve", lambda e, h=h: e.tensor_tensor(out=stg[:], in0=stg[:], in1=msk[:], op=ALU.add), [stg, msk], [stg])
            P.op("act", lambda e, h=h: e.activation(out=nbk[:, h], in_=stg[:], func=AF.Exp), [stg], [nbk])
        biasB = nbk
    else:
        swm = P.sb([128, 3, 128], BF16, "swm")
        swf32 = P.sb([128, 2, 128], F32, "swf32")
        P.dma("sp", lambda e: e.dma_start(out=swf32[:], in_=C.swmask.rearrange("k p q -> p k q")), [], [swf32])
        P.op("dve", lambda e: e.memset(swm[:], 1.0), [], [swm])
        P.op("act", lambda e: e.activation(out=swm[:, 0, :], in_=swf32[:, 0, :], func=AF.Exp), [swf32], [swm])
        P.op("act", lambda e: e.activation(out=swm[:, 2, :], in_=swf32[:, 1, :], func=AF.Exp), [swf32], [swm])
        esink = P.sb([128, 4], F32, "esink")
        P.dma("sp", lambda e: e.dma_start(out=esink[:], in_=C.swa_sink[l].partition_broadcast(128)), [], [esink])
        P.op("act", lambda e: e.activation(out=esink[:], in_=esink[:], func=AF.Exp), [esink], [esink])
        biasB = swm
    pts = [P.sb([128, 640], BF16, "pt") for _ in range(3)]
    den = P.sb([128, 4], F32, "den")
    otok = [P.sb([128, 4, 64], BF16, "otok") for _ in range(2)]
    sto = [P.sb([128, 2, 512], BF16, "sto") for _ in range(2)]
    spair = [Buf(None, "sp01"), Buf(None, "sp23")]
    ob = [bk[4], bk[5]]
    cbase = 0 if na else 6
    it = 0
    def s_stage(j, h, it):
        blocks = na_blocks(j) if na else sw_blocks(j)
        nb = len(blocks)
        sp_ = spair[it % 2]
        sbase = (it % 2) * 1024
        pt = pts[it % 3]
        pr = slice((h % 2) * 64, (h % 2) * 64 + 64)
        for bi, (kt, bkind) in enumerate(blocks):
            o_ap = ps[:, sbase + bi * 128: sbase + (bi + 1) * 128]
            _mm(P, o_ap, kT[pr, h // 2, kt * 128:(kt + 1) * 128], qT[pr, h // 2, j * 128:(j + 1) * 128], True, True, [kT, qT], [sp_])
        P.op("act", lambda e, pt=pt, sbase=sbase, nb=nb: e.activation(out=pt[:, 0:nb * 128], in_=ps[:, sbase: sbase + nb * 128], func=AF.Exp), [sp_], [pt])
        if na:
            k0 = blocks[0][1]
            eb = nbk[:, h, k0:k0 + nb, :].rearrange("p k q -> p (k q)")
        else:
            b0 = 1 if j == 0 else 0
            eb = swm[:, b0:b0 + nb, :].rearrange("p k q -> p (k q)")
        P.op("dve", lambda e, pt=pt, nb=nb, eb=eb: e.tensor_tensor(out=pt[:, 0:nb * 128], in0=pt[:, 0:nb * 128], in1=eb, op=ALU.mult), [pt, biasB], [pt])

    def pv_stage(j, h, it):
        blocks = na_blocks(j) if na else sw_blocks(j)
        nb = len(blocks)
        pt = pts[it % 3]
        jo = j % 2
        hv = h if na else h // 2
        for bi, (kt, bkind) in enumerate(blocks):
            _mm(P, ps[:, (4 + jo) * 512 + h * 65: (4 + jo) * 512 + (h + 1) * 65], pt[:, bi * 128:(bi + 1) * 128],
                v[:, kt, hv * 65:(hv + 1) * 65], bi == 0, bi == nb - 1, [pt, v], [ob[jo]])

    def fin_stage(j):
        ch, tt = j // 4, j % 4
        jo = j % 2
        ov = ps[:, (4 + jo) * 512: (4 + jo) * 512 + 260].rearrange("p (h d) -> p h d", h=4)
        dn = dens[jo]
        if na:
            P.op("dve", lambda e, ov=ov, dn=dn: e.reciprocal(out=dn[:], in_=ov[:, :, 64]), [ob[jo]], [dn])
        else:
            P.op("dve", lambda e, ov=ov, dn=dn: e.tensor_tensor(out=dn[:], in0=ov[:, :, 64], in1=esink[:], op=ALU.add), [ob[jo], esink], [dn])
            P.op("dve", lambda e, dn=dn: e.reciprocal(out=dn[:], in_=dn[:]), [dn], [dn])
        ot = otok[jo]
        P.op("dve", lambda e, ov=ov, ot=ot, dn=dn: e.tensor_tensor(out=ot[:], in0=ov[:, :, 0:64], in1=dn[:].unsqueeze(2).broadcast_to([128, 4, 64]), op=ALU.mult),
             [ob[jo], dn], [ot])
        otf = ot[:].rearrange("p h d -> p (h d)")
        tb = 6 + jo
        for cb in range(2):
            _tr(P, psb[:, tb * 1024 + cb * 128: tb * 1024 + (cb + 1) * 128], otf[:, cb * 128:(cb + 1) * 128], K.ident[:], [ot, K.ident], [bk[tb]])
        so = sto[ch % 2]
        P.op("act", lambda e, so=so, tt=tt, tb=tb: e.activation(out=so[:, :, tt * 128:(tt + 1) * 128],
                                                         in_=psb[:, tb * 1024: tb * 1024 + 256].rearrange("p (c t) -> p c t", c=2), func=AF.Copy), [bk[tb]], [so])
        if tt == 3:
            P.dma("sp", lambda e, so=so, ch=ch: e.dma_start(out=C.oT_d[cbase:cbase + 2, :, ch * 512:(ch + 1) * 512].rearrange("c p t -> p c t"), in_=so[:]),
                  [so], [C.B["oT_" + kind][ch]])

    dens = [den, P.sb([128, 4], F32, "den2")]
    items = [(j, h) for j in range(NT) for h in range(H)]
    s_stage(items[0][0], items[0][1], 0)
    for n, (j, h) in enumerate(items):
        if n + 1 < len(items):
            s_stage(items[n + 1][0], items[n + 1][1], n + 1)
        pv_stage(j, h, n)
        if h == H - 1:
            fin_stage(j)
    P.barrier()
    P.release(mk)


def phase_mla(P, C, K, l):
    mk = P.mark()
    ps, psb, bk = C.ps, C.psb, C.bk
    cqT = P.sb([128, 3, S], BF16, "cqT")
    KT = P.sb([128, 8, S], BF16, "KT")
    Vv = P.sb([128, NT, 8, 65], BF16, "Vv")
    w_q = P.sb([128, 3, 768], BF16, "w_q")
    w_qr = P.sb([128, 3, 768], BF16, "w_qr")
    for c in range(3):
        P.dma("sp", lambda e, c=c: e.dma_start(out=cqT[:, c, :], in_=C.cqT_d[c]), C.B["cqT"], [cqT])
    P.dma("pool", lambda e: e.dma_start(out=w_q[:], in_=C.mla_w_q_up[l].rearrange("(c p) n -> p c n", p=128)), [], [w_q])
    P.op("pool", lambda e: e.memset(w_qr[:], 0.0), [], [w_qr])
    wq4 = w_q[:].rearrange("p c (h d) -> p c h d", h=8)
    wr4 = w_qr[:].rearrange("p c (h d) -> p c h d", h=8)
    for c in range(3):
        P.op("act", lambda e, c=c: e.activation(out=wr4[:, c, :, 64:80], in_=wq4[:, c, :, 80:96], func=AF.Copy, scale=-1.0), [w_q], [w_qr])
        P.op("dve", lambda e, c=c: e.tensor_copy(out=wr4[:, c, :, 80:96], in_=wq4[:, c, :, 64:80]), [w_q], [w_qr])
    P.op("pool", lambda e: e.memset(Vv[:], 1.0), [], [Vv])
    for h in range(8):
        P.dma("sp", lambda e, h=h: e.dma_start(out=KT[64:96, h, :], in_=C.krT_d), C.B["krT"], [KT])
    mk2 = P.mark()
    ckvT = P.sb([128, 2, S], BF16, "ckvT")
    w_kv = P.sb([128, 2, 1024], BF16, "w_kv")
    for c in range(2):
        P.dma("sp", lambda e, c=c: e.dma_start(out=ckvT[:, c, :], in_=C.ckvT_d[c]), C.B["ckvT"], [ckvT])
    P.dma("pool", lambda e: e.dma_start(out=w_kv[:], in_=C.mla_w_kv_up[l].rearrange("(c p) n -> p c n", p=128)), [], [w_kv])
    wkv4 = w_kv[:].rearrange("p c (h a d) -> p c h a d", h=8, a=2)
    it = 0
    for c in range(NCH):
        for h in range(8):
            b = it % 4
            it += 1
            for kc in range(2):
                _mm(P, ps[0:64, b * 512:(b + 1) * 512], w_kv[:, kc, h * 128:h * 128 + 64], ckvT[:, kc, c * 512:(c + 1) * 512], kc == 0, kc == 1, [w_kv, ckvT], [bk[b]])
            if it % 2:
                P.op("act", lambda e, b=b, h=h, c=c: e.activation(out=KT[0:64, h, c * 512:(c + 1) * 512], in_=ps[0:64, b * 512:(b + 1) * 512], func=AF.Copy), [bk[b]], [KT])
            else:
                P.op("dve", lambda e, b=b, h=h, c=c: e.tensor_copy(out=KT[0:64, h, c * 512:(c + 1) * 512], in_=ps[0:64, b * 512:(b + 1) * 512]), [bk[b]], [KT])
    for T in range(NT):
        b = 4 + T % 2
        for kc in range(2):
            _mm(P, ps[:, b * 512:(b + 1) * 512], ckvT[:, kc, T * 128:(T + 1) * 128], wkv4[:, kc, :, 1, :], kc == 0, kc == 1, [w_kv, ckvT], [bk[b]])
        eng = "act" if T % 2 else "dve"
        if eng == "act":
            P.op("act", lambda e, b=b, T=T: e.activation(out=Vv[:, T, :, 0:64], in_=ps[:, b * 512:(b + 1) * 512].rearrange("p (h d) -> p h d", h=8), func=AF.Copy), [bk[b]], [Vv])
        else:
            P.op("dve", lambda e, b=b, T=T: e.tensor_copy(out=Vv[:, T, :, 0:64], in_=ps[:, b * 512:(b + 1) * 512].rearrange("p (h d) -> p h d", h=8)), [bk[b]], [Vv])
    P.barrier()
    P.release(mk2)
    cst = [P.sb([128, 512], F32, "cst") for _ in range(2)]
    snt = [P.sb([128, 512], F32, "snt") for _ in range(2)]
    QT = [P.sb([128, 512], BF16, "QT") for _ in range(2)]
    t1 = P.sb([128, 512], F32, "t1")
    t2 = P.sb([128, 512], F32, "t2")
    PT = [P.sb([128, 512], BF16, "PT") for _ in range(3)]
    rrow = P.sb([128, 512], F32, "rrow")
    bcs = P.sb([64, 512], F32, "bcs")
    sto = [P.sb([64, 512], BF16, "sto") for _ in range(2)]
    scale = float(96 ** -0.5)
    si = 0
    qi = 0
    def load_cs(c):
        cs = slice(c * 512, (c + 1) * 512)
        ct, sn = cst[c % 2], snt[c % 2]
        P.dma("sp", lambda e, ct=ct, cs=cs: e.dma_start(out=ct[64:96, :], in_=C.cosT_mla[:, cs]), [], [ct])
        P.dma("sp", lambda e, sn=sn, cs=cs: e.dma_start(out=sn[64:96, :], in_=C.sinT_mla[:, cs]), [], [sn])

    def qproj(i):
        c, h = i // 8, i % 8
        cs = slice(c * 512, (c + 1) * 512)
        ct, sn = cst[c % 2], snt[c % 2]
        q = QT[i % 2]
        for kc in range(3):
            _mm(P, ps[0:96, 5 * 512:6 * 512], w_q[:, kc, h * 96:(h + 1) * 96], cqT[:, kc, cs], kc == 0, kc == 2, [w_q, cqT], [bk[5]])
        for kc in range(3):
            _mm(P, ps[0:96, 6 * 512:7 * 512], w_qr[:, kc, h * 96:(h + 1) * 96], cqT[:, kc, cs], kc == 0, kc == 2, [w_qr, cqT], [bk[6]])
        P.op("dve", lambda e, q=q: e.tensor_copy(out=q[0:64, :], in_=ps[0:64, 5 * 512:6 * 512]), [bk[5]], [q])
        P.op("dve", lambda e, ct=ct: e.tensor_tensor(out=t1[64:96, :], in0=ps[64:96, 5 * 512:6 * 512], in1=ct[64:96, :], op=ALU.mult), [bk[5], ct], [t1])
        P.op("dve", lambda e, sn=sn: e.tensor_tensor(out=t2[64:96, :], in0=ps[64:96, 6 * 512:7 * 512], in1=sn[64:96, :], op=ALU.mult), [bk[6], sn], [t2])
        P.op("dve", lambda e, q=q: e.tensor_tensor(out=q[64:96, :], in0=t1[64:96, :], in1=t2[64:96, :], op=ALU.add), [t1, t2], [q])

    def tail_a(i):
        ob = 3 + i % 2
        P.op("dve", lambda e, ob=ob: e.reciprocal(out=rrow[64:65, :], in_=ps[64:65, ob * 512:(ob + 1) * 512]), [bk[ob]], [rrow])

    def tail(i):
        c, h = i // 8, i % 8
        cs = slice(c * 512, (c + 1) * 512)
        ob = 3 + i % 2
        so = sto[i % 2]
        _mm(P, ps[0:64, 7 * 512:8 * 512], K.onesf[64:65, 0:64], rrow[64:65, :], True, True, [K.onesf, rrow], [bk[7]])
        P.op("dve", lambda e: e.tensor_copy(out=bcs[:], in_=ps[0:64, 7 * 512:8 * 512]), [bk[7]], [bcs])
        P.op("dve", lambda e, so=so, ob=ob: e.tensor_tensor(out=so[:], in0=ps[0:64, ob * 512:(ob + 1) * 512], in1=bcs[:], op=ALU.mult), [bk[ob], bcs], [so])
        P.dma("sp", lambda e, so=so, h=h, cs=cs: e.dma_start(out=C.oT_d[2 + h // 2, (h % 2) * 64:(h % 2) * 64 + 64, cs], in_=so[:]), [so], [C.B["oT_mla"][c]])

    NI = NCH * 8
    load_cs(0)
    qproj(0)
    for i in range(NI):
        c, h = i // 8, i % 8
        q = QT[i % 2]
        ob = 3 + i % 2
        sbs = []
        for step in range(NT + 2):
            if step == 3 and h == 0 and c + 1 < NCH:
                load_cs(c + 1)
            if step == 4 and i >= 1:
                tail_a(i - 1)
            if step == 14 and i >= 1:
                tail(i - 1)
            if step == 20 and i + 1 < NI:
                qproj(i + 1)
            if step < NT:
                kt = step
                sb_ = si % 3
                pt = PT[si % 3]
                si += 1
                sbs.append((sb_, pt))
                _mm(P, ps[:, sb_ * 512:(sb_ + 1) * 512], KT[0:96, h, kt * 128:(kt + 1) * 128], q[0:96, :], True, True, [KT, q], [bk[sb_]])
                P.op("act", lambda e, pt=pt, sb_=sb_: e.activation(out=pt[:], in_=ps[:, sb_ * 512:(sb_ + 1) * 512], func=AF.Exp, scale=scale), [bk[sb_]], [pt])
            if step >= 2:
                kt = step - 2
                sb_, pt = sbs[kt]
                _mm(P, ps[0:65, ob * 512:(ob + 1) * 512], Vv[:, kt, h, :], pt[:], kt == 0, kt == NT - 1, [Vv, pt], [bk[ob]])
    tail_a(NI - 1)
    tail(NI - 1)
    P.barrier()
    P.release(mk)


def phase_O(P, C, K, l):
    mk = P.mark()
    ps, psb, bk = C.ps, C.psb, C.bk
    w_out = C.v_w_out
    w_r = C.v_w_r
    g_f = P.sb([128, 1024], F32, "g_f")
    P.dma("sp", lambda e: e.dma_start(out=g_f[:], in_=C.ffn_norm[l].partition_broadcast(128)), [], [g_f])
    oTs = [P.sb([128, 8, 512], BF16, "oTs") for _ in range(2)]
    hts = [P.sb([128, 1024], F32, "ht") for _ in range(2)]
    h2s = [P.sb([128, 1024], F32, "h2") for _ in range(2)]
    xns = [P.sb([128, 1024], BF16, "xn") for _ in range(2)]
    xTs = [P.sb([128, 8, 128], BF16, "xT") for _ in range(2)]
    junk = P.sb([128, 1024], BF16, "junk")
    ss = [P.sb([128, 1], F32, "ss") for _ in range(2)]
    rs = [P.sb([128, 1], F32, "rs") for _ in range(2)]
    exs = [P.sb([128, 16], F32, "ex") for _ in range(2)]
    sumes = [P.sb([128, 1], F32, "sume") for _ in range(2)]
    src = C.x if l == 0 else C.h_d
    srcB = C.B["x"] if l == 0 else C.B["h"]
    oTB = lambda ch: [C.B["oT_na"][ch], C.B["oT_mla"][ch], C.B["oT_sw"][ch]]
    def front(T):
        ch, tt = T // 4, T % 4
        oT = oTs[ch % 2]
        if tt == 0:
            P.dma("sp", lambda e, oT=oT, ch=ch: e.dma_start(out=oT[:], in_=C.oT_d[:, :, ch * 512:(ch + 1) * 512].rearrange("c p t -> p c t")), oTB(ch), [oT])
        ht, h2 = hts[T % 2], h2s[T % 2]
        P.dma("sp", lambda e, ht=ht, T=T: e.dma_start(out=ht[:], in_=src[T * 128:(T + 1) * 128, :]), [srcB[T]], [ht])
        for half in range(2):
            for k in range(8):
                _mm(P, ps[:, half * 512:(half + 1) * 512], oT[:, k, tt * 128:(tt + 1) * 128], w_out[:, k, half * 512:(half + 1) * 512], k == 0, k == 7, [oT, w_out], [bk[half]])
            P.op("dve", lambda e, half=half, ht=ht, h2=h2: e.tensor_tensor(out=h2[:, half * 512:(half + 1) * 512], in0=ps[:, half * 512:(half + 1) * 512],
                                                                          in1=ht[:, half * 512:(half + 1) * 512], op=ALU.add), [bk[half], ht], [h2])
        P.dma("sp", lambda e, h2=h2, T=T: e.dma_start(out=C.h_d[T * 128:(T + 1) * 128, :], in_=h2[:]), [h2], [C.B["h"][T]])

    def back(T):
        h2, xn, xT = h2s[T % 2], xns[T % 2], xTs[T % 2]
        P.op("act", lambda e, h2=h2, T=T: e.activation(out=junk[:], in_=h2[:], func=AF.Square, accum_out=ss[T % 2][:]), [h2], [junk, ss[T % 2]])
        _rstd(P, ss[T % 2], rs[T % 2], 1024)
        P.op("dve", lambda e, h2=h2, xn=xn, T=T: e.scalar_tensor_tensor(out=xn[:], in0=h2[:], scalar=rs[T % 2][:], in1=g_f[:], op0=ALU.mult, op1=ALU.mult),
             [h2, rs[T % 2], g_f], [xn])
        P.dma("sp", lambda e, xn=xn, T=T: e.dma_start(out=C.hnb_d[T * 128:(T + 1) * 128, :], in_=xn[:]), [xn], [C.B["hnb"][T]])
        tb = 5 + T % 2
        for c in range(8):
            _tr(P, psb[:, tb * 1024 + c * 128: tb * 1024 + (c + 1) * 128], xn[:, c * 128:(c + 1) * 128], K.ident[:], [xn, K.ident], [bk[tb]])
        P.op("act", lambda e, xT=xT, tb=tb: e.activation(out=xT[:].rearrange("p c t -> p (c t)"), in_=psb[:, tb * 1024: (tb + 1) * 1024], func=AF.Copy), [bk[tb]], [xT])
        rb_ = 2 + T % 2
        for k in range(8):
            _mm(P, ps[:, rb_ * 512: rb_ * 512 + 16], xT[:, k, :], w_r[:, k, :], k == 0, k == 7, [xT, w_r], [bk[rb_]])
        ex, sume = exs[T % 2], sumes[T % 2]
        P.op("act", lambda e, ex=ex, sume=sume, rb_=rb_: e.activation(out=ex[:], in_=ps[:, rb_ * 512: rb_ * 512 + 16], func=AF.Exp, accum_out=sume[:]), [bk[rb_]], [ex, sume])
        P.op("dve", lambda e, sume=sume: e.reciprocal(out=sume[:], in_=sume[:]), [sume], [sume])
        P.op("dve", lambda e, T=T, ex=ex, sume=sume: e.tensor_scalar(out=K.aff[:, T, :], in0=ex[:], scalar1=sume[:], scalar2=None, op0=ALU.mult), [ex, sume], [K.aff])

    for T in range(NT + 1):
        if T < NT:
            front(T)
        if T >= 1:
            back(T - 1)
    P.barrier()
    P.release(mk)


def phase_R(P, C, K, l):
    mk = P.mark()
    pre_exp0(P, C, l)
    ps, psb, bk = C.ps, C.psb, C.bk
    lo = P.sb([128, 16], F32, "lo")
    mid = P.sb([128, 16], F32, "mid")
    cnt = P.sb([128, 16], F32, "cnt")
    m_ = P.sb([128, 16], F32, "m_")
    cmp_ = [P.sb([128, 32, 16], BF16, "cmp") for _ in range(2)]
    aff = K.aff
    P.op("dve", lambda e: e.memset(lo[:], 0.0), [], [lo])
    NIT = 26
    for it in range(NIT):
        w = 2.0 ** -(it + 1)
        cm = cmp_[it % 2]
        b = it % 2
        P.op("dve", lambda e, w=w: e.tensor_scalar(out=mid[:], in0=lo[:], scalar1=w, scalar2=None, op0=ALU.add), [lo], [mid])
        P.op("dve", lambda e, cm=cm: e.tensor_tensor(out=cm[:], in0=aff[:], in1=mid[:].unsqueeze(1).broadcast_to([128, 32, 16]), op=ALU.is_gt), [aff, mid], [cm])
        _mm(P, ps[:, b * 512:(b + 1) * 512], K.ones[:], cm[:].rearrange("p i e -> p (i e)"), True, True, [K.ones, cm], [bk[b]])
        P.op("dve", lambda e, b=b: e.tensor_reduce(out=cnt[:], in_=ps[:, b * 512:(b + 1) * 512].rearrange("p (i e) -> p e i", e=16), axis=AX.X, op=ALU.add), [bk[b]], [cnt])
        P.op("dve", lambda e: e.tensor_scalar(out=m_[:], in0=cnt[:], scalar1=511.5, scalar2=None, op0=ALU.is_gt), [cnt], [m_])
        P.op("dve", lambda e, w=w: e.scalar_tensor_tensor(out=lo[:], in0=m_[:], scalar=w, in1=lo[:], op0=ALU.mult, op1=ALU.add), [m_, lo], [lo])
    mask = P.sb([128, 32, 16], F32, "mask")
    maskb = P.sb([128, 32, 16], BF16, "maskb")
    gm = P.sb([128, 32, 16], F32, "gm")
    P.op("dve", lambda e: e.tensor_tensor(out=mask[:], in0=aff[:], in1=lo[:].unsqueeze(1).broadcast_to([128, 32, 16]), op=ALU.is_gt), [aff, lo], [mask])
    P.op("dve", lambda e: e.tensor_copy(out=maskb[:], in_=mask[:]), [mask], [maskb])
    P.op("dve", lambda e: e.tensor_tensor(out=gm[:], in0=aff[:], in1=mask[:], op=ALU.mult), [aff, mask], [gm])
    mflat = maskb[:].rearrange("p i e -> p (i e)")
    _mm(P, ps[:, 0:512], K.ones[:], mflat, True, True, [K.ones, maskb], [bk[0]])
    _mm(P, ps[:, 512:1024], K.L[:], mflat, True, True, [K.L, maskb], [bk[1]])
    cnt_ei = P.sb([128, 16, 32], F32, "cnt_ei")
    cum = P.sb([128, 16, 32], F32, "cum")
    rst = P.sb([128, 16, 32], F32, "rst")
    pos = P.sb([128, 32, 16], F32, "pos")
    tmp = P.sb([128, 32, 16], F32, "tmp")
    P.op("dve", lambda e: e.tensor_copy(out=cnt_ei[:], in_=ps[:, 0:512].rearrange("p (i e) -> p e i", e=16)), [bk[0]], [cnt_ei])
    P.op("pool", lambda e: e.memset(rst[:], 1.0), [], [rst])
    P.op("pool", lambda e: e.memset(rst[:, :, 0:1], 0.0), [rst], [rst])
    P.op("dve", lambda e: e.tensor_tensor_scan(out=cum[:].rearrange("p e i -> p (e i)"), data0=rst[:].rearrange("p e i -> p (e i)"),
                                               data1=cnt_ei[:].rearrange("p e i -> p (e i)"), initial=0.0, op0=ALU.mult, op1=ALU.add), [rst, cnt_ei], [cum])
    P.op("dve", lambda e: e.tensor_tensor(out=cum[:], in0=cum[:], in1=cnt_ei[:], op=ALU.subtract), [cum, cnt_ei], [cum])
    P.op("dve", lambda e: e.tensor_tensor(out=pos[:], in0=ps[:, 512:1024].rearrange("p (i e) -> p i e", e=16), in1=cum[:].rearrange("p e i -> p i e"), op=ALU.add),
         [bk[1], cum], [pos])
    P.op("dve", lambda e: e.tensor_tensor(out=pos[:], in0=pos[:], in1=mask[:], op=ALU.mult), [pos, mask], [pos])
    P.op("dve", lambda e: e.tensor_scalar(out=tmp[:], in0=mask[:], scalar1=-1.0, scalar2=None, op0=ALU.add), [mask], [tmp])
    P.op("dve", lambda e: e.tensor_tensor(out=pos[:], in0=pos[:], in1=tmp[:], op=ALU.add), [pos, tmp], [pos])
    vals = P.sb([128, 32, 16, 4], BF16, "vals")
    ghi = P.sb([128, 32, 16], BF16, "ghi")
    icol = P.sb([128, 32], F32, "icol")
    P.op("dve", lambda e: e.tensor_copy(out=icol[:], in_=K.iota[:, 0:32]), [K.iota], [icol])
    P.op("dve", lambda e: e.tensor_copy(out=vals[:, :, :, 0], in_=K.pcol[:].unsqueeze(2).broadcast_to([128, 32, 16])), [K.pcol], [vals])
    P.op("dve", lambda e: e.tensor_copy(out=vals[:, :, :, 1], in_=icol[:].unsqueeze(2).broadcast_to([128, 32, 16])), [icol], [vals])
    P.op("dve", lambda e: e.tensor_copy(out=ghi[:], in_=gm[:]), [gm], [ghi])
    P.op("dve", lambda e: e.tensor_copy(out=vals[:, :, :, 2], in_=ghi[:]), [ghi], [vals])
    P.op("dve", lambda e: e.tensor_tensor(out=vals[:, :, :, 3], in0=gm[:], in1=ghi[:], op=ALU.subtract), [gm, ghi], [vals])
    Pt = [P.sb([128, 512], BF16, "Pt") for _ in range(4)]
    rsb = [P.sb([4, 512], F32, "rsb") for _ in range(2)]
    tvs = [P.sb([128, 16], F32, "tvs") for _ in range(2)]
    pi = 0
    for ex_ in range(16):
        rb_ = 2 + ex_ % 2
        for i in range(NT):
            pt = Pt[pi % 4]
            eng = "dve"
            pi += 1
            P.op(eng, lambda e, pt=pt, i=i, ex_=ex_: e.tensor_scalar(out=pt[:], in0=K.iota16[:], scalar1=pos[:, i, ex_:ex_ + 1], scalar2=None, op0=ALU.is_equal), [K.iota16, pos], [pt])
            _mm(P, ps[0:4, rb_ * 512:(rb_ + 1) * 512], vals[:, i, ex_, :], pt[:], i == 0, i == NT - 1, [vals, pt], [bk[rb_]])
        r_ = rsb[ex_ % 2]
        P.op("act", lambda e, r_=r_, rb_=rb_: e.activation(out=r_[:], in_=ps[0:4, rb_ * 512:(rb_ + 1) * 512], func=AF.Copy), [bk[rb_]], [r_])
        tb = 4 + ex_ % 2
        for s_ in range(4):
            _tr(P, ps[:, tb * 512 + s_ * 4: tb * 512 + s_ * 4 + 4], r_[:, s_ * 128:(s_ + 1) * 128], K.identf[0:4, 0:4], [r_, K.identf], [bk[tb]])
        tvb = tvs[ex_ % 2]
        P.op("act", lambda e, tvb=tvb, tb=tb: e.activation(out=tvb[:], in_=ps[:, tb * 512: tb * 512 + 16], func=AF.Copy), [bk[tb]], [tvb])
        tv = tvb[:].rearrange("p (s f) -> p s f", f=4)
        P.op("dve", lambda e, tv=tv, ex_=ex_: e.scalar_tensor_tensor(out=K.idx[:, ex_, :], in0=tv[:, :, 1], scalar=128.0, in1=tv[:, :, 0], op0=ALU.mult, op1=ALU.add), [tvb], [K.idx])
        P.op("dve", lambda e, tv=tv, ex_=ex_: e.tensor_tensor(out=K.gsl[:, ex_, :], in0=tv[:, :, 2], in1=tv[:, :, 3], op=ALU.add), [tvb], [K.gsl])
    P.barrier()
    P.release(mk)


def phase_E(P, C, K, l):
    mk = P.mark()
    ps, psb, bk = C.ps, C.psb, C.bk
    wg = [P.sb([128, 8, 512], BF16, "wg") for _ in range(2)]
    wu = [P.sb([128, 8, 512], BF16, "wu") for _ in range(2)]
    wd = [P.sb([128, 4, 1024], BF16, "wd") for _ in range(2)]
    xs = [[P.sb([128, 1024], BF16, "xs") for _ in range(4)] for _ in range(2)]
    xsT = P.sb([128, 8, 512], BF16, "xsT")
    sa = [P.sb([128, 512], F32, "sa") for _ in range(2)]
    hT = P.sb([128, 4, 512], BF16, "hT")
    ys = [P.sb([128, 1024], F32, "y") for _ in range(2)]
    hB = C.B["h"]
    hnB = C.B["hnb"]
    hsc = [[Buf(None, "hsc%d_%d" % (a, b)) for b in range(4)] for a in range(2)]

    def loads(ex_):
        o = ex_ % 2
        if ex_ > 0:
            P.dma("pool", lambda e: e.dma_start(out=wg[o][:], in_=C.w_gate_e[l, ex_].rearrange("(c p) n -> p c n", p=128)), [], [wg[o]])
            P.dma("pool", lambda e: e.dma_start(out=wu[o][:], in_=C.w_up_e[l, ex_].rearrange("(c p) n -> p c n", p=128)), [], [wu[o]])
            P.dma("pool", lambda e: e.dma_start(out=wd[o][:], in_=C.w_down_e[l, ex_].rearrange("(c p) n -> p c n", p=128)), [], [wd[o]])
        for s_ in range(4):
            P.dma("pool", lambda e, s_=s_: e.indirect_dma_start(out=xs[o][s_][:], out_offset=None, in_=C.hnb_d,
                                                               in_offset=bass.IndirectOffsetOnAxis(ap=K.idx[:, ex_, s_:s_ + 1], axis=0)),
                  [K.idx] + hnB, [xs[o][s_]])
    loads(0)
    yi = 0
    for ex_ in range(16):
        o = ex_ % 2
        if ex_ + 1 < 16:
            loads(ex_ + 1)
        if ex_ == 1:
            pre_ple(P, C, l)
        wg_e, wu_e, wd_e = (C.v_wg0, C.v_wu0, C.v_wd0) if ex_ == 0 else (wg[o], wu[o], wd[o])
        for s_ in range(4):
            tb = 6 + s_ % 2
            for c in range(8):
                _tr(P, psb[:, tb * 1024 + c * 128: tb * 1024 + (c + 1) * 128], xs[o][s_][:, c * 128:(c + 1) * 128], K.ident[:], [xs[o][s_], K.ident], [bk[tb]])
            P.op("act", lambda e, s_=s_, tb=tb: e.activation(out=xsT[:, :, s_ * 128:(s_ + 1) * 128], in_=psb[:, tb * 1024:(tb + 1) * 1024].rearrange("p (c t) -> p c t", c=8), func=AF.Copy), [bk[tb]], [xsT])
        for f in range(4):
            ba, bu = (f % 2) * 2, (f % 2) * 2 + 1
            for k in range(8):
                _mm(P, ps[:, ba * 512:(ba + 1) * 512], wg_e[:, k, f * 128:(f + 1) * 128], xsT[:, k, :], k == 0, k == 7, [wg_e, xsT], [bk[ba]])
            for k in range(8):
                _mm(P, ps[:, bu * 512:(bu + 1) * 512], wu_e[:, k, f * 128:(f + 1) * 128], xsT[:, k, :], k == 0, k == 7, [wu_e, xsT], [bk[bu]])
            s1 = sa[f % 2]
            P.op("act", lambda e, s1=s1, ba=ba: e.activation(out=s1[:], in_=ps[:, ba * 512:(ba + 1) * 512], func=AF.Silu), [bk[ba]], [s1])
            P.op("dve", lambda e, s1=s1, bu=bu, f=f: e.tensor_tensor(out=hT[:, f, :], in0=ps[:, bu * 512:(bu + 1) * 512], in1=s1[:], op=ALU.mult), [bk[bu], s1], [hT])
        for s_ in range(4):
            y = ys[yi % 2]
            yi += 1
            for half in range(2):
                bb = 4 + half
                for f in range(4):
                    _mm(P, ps[:, bb * 512:(bb + 1) * 512], hT[:, f, s_ * 128:(s_ + 1) * 128], wd_e[:, f, half * 512:(half + 1) * 512], f == 0, f == 3, [hT, wd_e], [bk[bb]])
                if half:
                    P.op("act", lambda e, y=y, bb=bb, s_=s_, ex_=ex_: e.activation(out=y[:, 512:1024], in_=ps[:, bb * 512:(bb + 1) * 512], func=AF.Copy, scale=K.gsl[:, ex_, s_:s_ + 1]), [bk[bb], K.gsl], [y])
                else:
                    P.op("dve", lambda e, y=y, bb=bb, s_=s_, ex_=ex_: e.tensor_scalar(out=y[:, 0:512], in0=ps[:, bb * 512:(bb + 1) * 512], scalar1=K.gsl[:, ex_, s_:s_ + 1], scalar2=None, op0=ALU.mult), [bk[bb], K.gsl], [y])
            P.dma("pool", lambda e, y=y, s_=s_, ex_=ex_: e.indirect_dma_start(out=C.h_d, out_offset=bass.IndirectOffsetOnAxis(ap=K.idx[:, ex_, s_:s_ + 1], axis=0),
                                                                    in_=y[:], in_offset=None, compute_op=ALU.add),
                  [y, K.idx] + (hsc[(ex_ + 1) % 2] if ex_ > 0 else hB), [hsc[ex_ % 2][s_]])
    P.barrier()
    P.release(mk)


def phase_P(P, C, K, l, last):
    mk = P.mark()
    ps, psb, bk = C.ps, C.psb, C.bk
    w_g = C.v_w_g
    w_p = C.v_w_p
    g_p = P.sb([128, 1024], F32, "g_p")
    if not last:
        pre_w_in(P, C, l + 1)
    P.dma("sp", lambda e: e.dma_start(out=g_p[:], in_=C.ple_norm[l].partition_broadcast(128)), [], [g_p])
    if last:
        g_fin = P.sb([128, 1024], F32, "g_fin")
        P.dma("sp", lambda e: e.dma_start(out=g_fin[:], in_=C.final_norm.partition_broadcast(128)), [], [g_fin])
    hts = [P.sb([128, 1024], F32, "ht") for _ in range(2)]
    pts = [P.sb([128, 256], BF16, "ptile") for _ in range(2)]
    xns = [P.sb([128, 1024], BF16, "xn") for _ in range(2)]
    xTs = [P.sb([128, 10, 128], BF16, "xT") for _ in range(2)]
    junk = P.sb([128, 1024], BF16, "junk")
    egs = [P.sb([128, 1024], F32, "eg") for _ in range(2)]
    pps = [P.sb([128, 1024], F32, "pp") for _ in range(2)]
    junk2 = P.sb([128, 1024], BF16, "junk2")
    h3s = [P.sb([128, 1024], F32, "h3") for _ in range(2)]
    outs = [P.sb([128, 1024], F32, "outt") for _ in range(2)]
    ss = [P.sb([128, 1], F32, "ss") for _ in range(2)]
    rs = [P.sb([128, 1], F32, "rs") for _ in range(2)]
    ss2 = [P.sb([128, 1], F32, "ss2") for _ in range(2)]
    rs2 = [P.sb([128, 1], F32, "rs2") for _ in range(2)]
    def front(T):
        ht, pt, xn, xT = hts[T % 2], pts[T % 2], xns[T % 2], xTs[T % 2]
        rows = slice(T * 128, (T + 1) * 128)
        P.dma("sp", lambda e, ht=ht, rows=rows: e.dma_start(out=ht[:], in_=C.h_d[rows, :]), [C.B["h"][T]], [ht])
        P.dma("pool", lambda e, pt=pt, rows=rows: e.dma_start(out=pt[:], in_=C.p[l, rows, :]), [], [pt])
        P.op("act", lambda e, ht=ht, T=T: e.activation(out=junk[:], in_=ht[:], func=AF.Square, accum_out=ss[T % 2][:]), [ht], [junk, ss[T % 2]])
        _rstd(P, ss[T % 2], rs[T % 2], 1024)
        P.op("dve", lambda e, ht=ht, xn=xn, T=T: e.scalar_tensor_tensor(out=xn[:], in0=ht[:], scalar=rs[T % 2][:], in1=g_p[:], op0=ALU.mult, op1=ALU.mult),
             [ht, rs[T % 2], g_p], [xn])

    def front_b(T):
        pt, xn, xT = pts[T % 2], xns[T % 2], xTs[T % 2]
        tb = 6 + T % 2
        for c in range(8):
            _tr(P, psb[:, tb * 1024 + c * 128: tb * 1024 + (c + 1) * 128], xn[:, c * 128:(c + 1) * 128], K.ident[:], [xn, K.ident], [bk[tb]])
        P.op("act", lambda e, xT=xT, tb=tb: e.activation(out=xT[:, 0:8, :].rearrange("p c t -> p (c t)"), in_=psb[:, tb * 1024:(tb + 1) * 1024], func=AF.Copy), [bk[tb]], [xT])
        for c in range(2):
            _tr(P, psb[:, 4 * 1024 + c * 128: 4 * 1024 + (c + 1) * 128], pt[:, c * 128:(c + 1) * 128], K.ident[:], [pt, K.ident], [bk[4]])
        P.op("act", lambda e, xT=xT: e.activation(out=xT[:, 8:10, :].rearrange("p c t -> p (c t)"), in_=psb[:, 4 * 1024: 4 * 1024 + 256], func=AF.Copy), [bk[4]], [xT])

    def back_mm(T):
        xT = xTs[T % 2]
        for half in range(2):
            for k in range(8):
                _mm(P, ps[:, half * 512:(half + 1) * 512], xT[:, k, :], w_g[:, k, half * 512:(half + 1) * 512], k == 0, k == 7, [xT, w_g], [bk[half]])
            for k in range(2):
                _mm(P, ps[:, (2 + half) * 512:(3 + half) * 512], xT[:, 8 + k, :], w_p[:, k, half * 512:(half + 1) * 512], k == 0, k == 1, [xT, w_p], [bk[2 + half]])
        for half in range(2):
            hs = slice(half * 512, (half + 1) * 512)
            eg = egs[T % 2]
            pp = pps[T % 2]
            P.op("act", lambda e, half=half, hs=hs, eg=eg: e.activation(out=eg[:, hs], in_=ps[:, half * 512:(half + 1) * 512], func=AF.Exp, scale=-1.0), [bk[half]], [eg])
            P.op("act", lambda e, hs=hs, eg=eg: e.activation(out=eg[:, hs], in_=eg[:, hs], func=AF.Ln, bias=1.0), [eg], [eg])
            P.op("act", lambda e, hs=hs, eg=eg: e.activation(out=eg[:, hs], in_=eg[:, hs], func=AF.Exp, scale=-1.0), [eg], [eg])

    def back(T):
        ht, xT, h3 = hts[T % 2], xTs[T % 2], h3s[T % 2]
        eg = egs[T % 2]
        pp = pps[T % 2]
        rows = slice(T * 128, (T + 1) * 128)
        for half in range(2):
            hs = slice(half * 512, (half + 1) * 512)
            P.op("dve", lambda e, half=half, hs=hs, eg=eg: e.tensor_tensor(out=eg[:, hs], in0=ps[:, (2 + half) * 512:(3 + half) * 512], in1=eg[:, hs], op=ALU.mult), [bk[2 + half], eg], [eg])
            P.op("dve", lambda e, hs=hs, ht=ht, h3=h3, eg=eg: e.tensor_tensor(out=h3[:, hs], in0=eg[:, hs], in1=ht[:, hs], op=ALU.add), [eg, ht], [h3])
        if not last:
            P.dma("sp", lambda e, h3=h3, rows=rows: e.dma_start(out=C.h_d[rows, :], in_=h3[:]), [h3], [C.B["h"][T]])
        else:
            ot = outs[T % 2]
            P.op("act", lambda e, h3=h3, T=T: e.activation(out=junk2[:], in_=h3[:], func=AF.Square, accum_out=ss2[T % 2][:]), [h3], [junk2, ss2[T % 2]])
            _rstd(P, ss2[T % 2], rs2[T % 2], 1024)
            P.op("dve", lambda e, h3=h3, ot=ot, T=T: e.scalar_tensor_tensor(out=ot[:], in0=h3[:], scalar=rs2[T % 2][:], in1=g_fin[:], op0=ALU.mult, op1=ALU.mult),
                 [h3, rs2[T % 2], g_fin], [ot])
            P.dma("sp", lambda e, ot=ot, rows=rows: e.dma_start(out=C.out[rows, :], in_=ot[:]), [ot], [C.B["out"][T]])

    front(0)
    front_b(0)
    for T in range(NT):
        if T + 1 < NT:
            front(T + 1)
        back_mm(T)
        if T + 1 < NT:
            front_b(T + 1)
        back(T)
    P.barrier()
    P.release(mk)


W_NAMES = [("attn_norm", [2, 1024]), ("w_in", [2, 1024, 1952]), ("mla_q_norm", [2, 384]), ("mla_w_q_up", [2, 384, 768]),
           ("mla_kv_norm", [2, 256]), ("mla_w_kv_up", [2, 256, 1024]), ("swa_sink", [2, 4]), ("w_out", [2, 1024, 1024]),
           ("ffn_norm", [2, 1024]), ("w_router", [2, 1024, 16]), ("w_gate_e", [2, 16, 1024, 512]), ("w_up_e", [2, 16, 1024, 512]),
           ("w_down_e", [2, 16, 512, 1024]), ("ple_norm", [2, 1024]), ("w_ple_gate", [2, 1024, 1024]), ("w_ple_proj", [2, 256, 1024]),
           ("final_norm", [1024])]
CONSTS = [("cos_swa", [S, 32]), ("sin_swa", [S, 32]), ("cos_mla", [S, 16]), ("sin_mla", [S, 16]), ("cosT_mla", [32, S]),
          ("sinT_mla", [32, S]), ("nab", [2, 4, 21, 128, 128]), ("namask", [21, 128, 128]), ("swmask", [2, 128, 128])]
SCRATCH = [("h_d", [S, D], F32), ("hnb_d", [S, D], BF16), ("naqT_d", [2, 128, S], BF16), ("nakT_d", [2, 128, S], BF16),
           ("nav_d", [S, 260], BF16), ("cqT_d", [3, 128, S], BF16), ("ckvT_d", [2, 128, S], BF16), ("krT_d", [32, S], BF16),
           ("swqT_d", [2, 128, S], BF16), ("swkT_d", [2, 128, S], BF16), ("swv_d", [S, 130], BF16), ("oT_d", [8, 128, S], BF16)]
PHASES = ["A", "na", "sw", "mla", "O", "R", "E", "P"]


def build(stop=None, dbg=()):
    nc = bass.Bass("TRN2", target_bir_lowering=False)
    C = NS()
    C.x = nc.dram_tensor("x", [S, D], F32, kind="ExternalInput").ap()
    C.p = nc.dram_tensor("p", [2, S, 256], F32, kind="ExternalInput").ap()
    for n, shp in W_NAMES + CONSTS:
        setattr(C, n, nc.dram_tensor(n, list(shp), F32, kind="ExternalInput").ap())
    C.out = nc.dram_tensor("out", [S, D], F32, kind="ExternalOutput").ap()
    for n, shp, dt_ in SCRATCH:
        kind = "ExternalOutput" if n in dbg else "Internal"
        setattr(C, n, nc.dram_tensor(n, list(shp), dt_, kind=kind).ap())
    if "aff_dbg" in dbg:
        C.aff_dbg = nc.dram_tensor("aff_dbg", [128, 512], F32, kind="ExternalOutput").ap()
        C.idx_dbg = nc.dram_tensor("idx_dbg", [128, 64], I32, kind="ExternalOutput").ap()
        C.gsl_dbg = nc.dram_tensor("gsl_dbg", [128, 64], F32, kind="ExternalOutput").ap()
    C.B = {}
    for n in ("x", "h", "hnb", "out"):
        C.B[n] = [Buf(None, n + str(i)) for i in range(NT)]
    for n in ("naqT", "nakT", "nav", "cqT", "ckvT", "krT", "swqT", "swkT", "swv", "oT_na", "oT_mla", "oT_sw"):
        C.B[n] = [Buf(None, n + str(i)) for i in range(NCH)]
    with ExitStack() as es:
        P = Prog(nc, es)
        C.ps = es.enter_context(nc.psum_tensor("ps", [128, 4096], F32))
        C.psb = C.ps.bitcast(BF16)
        C.bk = [Buf(None, "bank%d" % i) for i in range(8)]
        setup_regions(P, C)
        pre_w_in(P, C, 0)
        K = setup_consts(P, C)
        done = False
        for l in range(2):
            for ph in PHASES:
                if ph == "A":
                    phase_A(P, C, K, l)
                elif ph in ("na", "sw"):
                    phase_local(P, C, K, l, ph)
                elif ph == "mla":
                    phase_mla(P, C, K, l)
                elif ph == "O":
                    phase_O(P, C, K, l)
                elif ph == "R":
                    phase_R(P, C, K, l)
                    if "aff_dbg" in dbg and l == 0:
                        P.dma("sp", lambda e: e.dma_start(out=C.aff_dbg, in_=K.aff[:].rearrange("p i e -> p (i e)")), [K.aff], [])
                        P.dma("sp", lambda e: e.dma_start(out=C.idx_dbg, in_=K.idx[:].rearrange("p e s -> p (e s)")), [K.idx], [])
                        P.dma("sp", lambda e: e.dma_start(out=C.gsl_dbg, in_=K.gsl[:].rearrange("p e s -> p (e s)")), [K.gsl], [])
                elif ph == "E":
                    phase_E(P, C, K, l)
                elif ph == "P":
                    phase_P(P, C, K, l, l == 1)
                if stop == (l, ph):
                    done = True
                    break
            if done:
                break
        P.finish()
        print("n_inst", P.n_inst, "sbuf_hi", P.sb_hi, flush=True)
    return nc


def _rope_tab(dim):
    inv = (1.0 / (10000.0 ** (np.arange(0, dim, 2, dtype=np.float32) / np.float32(dim)))).astype(np.float32)
    ang = np.arange(S, dtype=np.float32)[:, None] * inv[None, :]
    return np.cos(ang).astype(np.float32), np.sin(ang).astype(np.float32)


def _na_tables():
    W = 64
    kl = np.arange(128)
    krl, kc = kl // W, kl % W
    qrl, qc = kl // W, kl % W
    dri = np.zeros((21, 128, 128), np.int64)
    dci = np.zeros((21, 128, 128), np.int64)
    msk = np.zeros((21, 128, 128), np.float32)
    for kd, (j, d) in enumerate(NA_KINDS):
        kt = j + d
        qr = 2 * j + qrl[None, :]
        kr = 2 * kt + krl[:, None]
        r0 = np.clip(qr - 4, 0, 56)
        row_ok = (kr >= r0) & (kr < r0 + 8)
        c0 = np.clip(qc[None, :] - 8, 0, 48)
        col_ok = (kc[:, None] >= c0) & (kc[:, None] < c0 + 16)
        dri[kd] = np.clip(kr - qr + 7, 0, 14)
        dci[kd] = np.clip(kc[:, None] - qc[None, :], -15, 15) + 15
        msk[kd] = np.where(row_ok & col_ok, 0.0, NEG)
    return dri, dci, msk


def _sw_masks():
    kl = np.arange(128)[:, None]
    ql = np.arange(128)[None, :]
    m = np.zeros((2, 128, 128), np.float32)
    m[0] = np.where(kl >= ql, 0.0, NEG)
    m[1] = np.where(kl <= ql, 0.0, NEG)
    return m


_NC_CACHE = {}


def host_inputs(inputs, cores):
    cs, sn = _rope_tab(64)
    cm, sm = _rope_tab(32)
    dri, dci, msk = _na_tables()
    rpb = np.asarray(inputs["na_rpb"], np.float32)
    nab = np.ascontiguousarray(rpb[:, :, dri, dci])
    shared = {n: np.ascontiguousarray(np.asarray(inputs[n], np.float32)) for n, _ in W_NAMES}
    shared.update(cos_swa=cs, sin_swa=sn, cos_mla=cm, sin_mla=sm,
                  cosT_mla=np.ascontiguousarray(np.concatenate([cm, cm], 1).T),
                  sinT_mla=np.ascontiguousarray(np.concatenate([sm, sm], 1).T),
                  nab=nab, namask=msk, swmask=_sw_masks())
    x = np.asarray(inputs["x"], np.float32)
    p = np.asarray(inputs["p"], np.float32)
    maps = []
    for b in cores:
        m = dict(shared)
        m["x"] = np.ascontiguousarray(x[b])
        m["p"] = np.ascontiguousarray(p[:, b])
        maps.append(m)
    return maps


def kernel(**inputs):
    if "nc" not in _NC_CACHE:
        _NC_CACHE["nc"] = build()
    nc = _NC_CACHE["nc"]
    maps = host_inputs(inputs, list(range(8)))
    res = run_bass_kernel_spmd(nc, maps, core_ids=list(range(8)))
    return np.stack([np.asarray(r["out"], np.float32) for r in res.results], axis=0)
```
